# Optimizing a Trainium2 kernel written in Bass

```python
import math
import jax, jax.numpy as jnp
from jax import lax
import numpy as np

D_MODEL = 1024
BATCH = 32
SEQ = 2048
DEPTH = 2

RET_HEADS = 4
RET_QK_DIM = 64
RET_V_DIM = 128
RET_CHUNK = 128
DIFF_HEADS = 4
DIFF_HEAD_DIM = 64
DIFF_V_DIM = 2 * DIFF_HEAD_DIM
Q_BLOCK = 128
FFN_HIDDEN = ((int(math.ceil(8 * D_MODEL / 3)) + 255) // 256) * 256
NORM_EPS = 1e-6

RET_Q_W = RET_HEADS * RET_QK_DIM
RET_V_W = RET_HEADS * RET_V_DIM
DIFF_QK_W = DIFF_HEADS * 2 * DIFF_HEAD_DIM
DIFF_V_W = DIFF_HEADS * DIFF_V_DIM
IN_SPLITS = [RET_Q_W, RET_Q_W, RET_V_W, RET_V_W, DIFF_QK_W, DIFF_QK_W, DIFF_V_W]
IN_WIDTH = sum(IN_SPLITS)
MIX_WIDTH = RET_V_W + DIFF_V_W

kernel_name = "hybrid_retention_diffattn_alibi"


def rmsnorm(x, g):
    xf = x.astype(jnp.float32)
    y = xf * lax.rsqrt(jnp.mean(xf * xf, axis=-1, keepdims=True) + NORM_EPS)
    return (y * g.astype(jnp.float32)).astype(x.dtype)


def retention_gammas():
    return 1.0 - jnp.exp2(-5.0 - jnp.arange(RET_HEADS, dtype=jnp.float32))


def retention_chunkwise(q, k, v):
    B, S, H, dk = q.shape
    dv = v.shape[-1]
    C = RET_CHUNK
    N = S // C
    dt = q.dtype
    log_g = jnp.log(retention_gammas())
    k = k * jnp.asarray(dk ** -0.5, dt)
    q = q.reshape(B, N, C, H, dk)
    k = k.reshape(B, N, C, H, dk)
    v = v.reshape(B, N, C, H, dv)
    idx = jnp.arange(C, dtype=jnp.float32)
    rel = idx[:, None] - idx[None, :]
    decay = jnp.where(rel[None] >= 0,
                      jnp.exp(jnp.maximum(rel, 0.0)[None] * log_g[:, None, None]),
                      0.0).astype(dt)
    s = jnp.einsum('bnchd,bnshd->bnhcs', q, k) * decay
    inner = jnp.einsum('bnhcs,bnshe->bnche', s, v)
    k_decay = jnp.exp((C - 1 - idx)[:, None] * log_g[None, :]).astype(dt)
    kv = jnp.einsum('bnshd,bnshe->nbhde', k * k_decay[:, :, None], v)
    g_chunk = jnp.exp(C * log_g).astype(dt)[None, :, None, None]

    def step(state, kv_n):
        return g_chunk * state + kv_n, state

    _, states = lax.scan(step, jnp.zeros((B, H, dk, dv), dt), kv)
    q_decay = jnp.exp((idx + 1.0)[:, None] * log_g[None, :]).astype(dt)
    cross = jnp.einsum('bnchd,nbhde->bnche', q * q_decay[:, :, None], states)
    return (inner + cross).reshape(B, S, H, dv)


def alibi_slopes(n):
    return jnp.exp2(-8.0 * jnp.arange(1, n + 1, dtype=jnp.float32) / n)


def diff_attention(q, k, v, lam):
    S = q.shape[1]
    d = q.shape[-1]
    scale = d ** -0.5
    slopes = alibi_slopes(q.shape[2])
    outs = []
    for i in range(S // Q_BLOCK):
        q0 = i * Q_BLOCK
        kend = q0 + Q_BLOCK
        qb = q[:, q0:kend]
        kb = k[:, :kend]
        vb = v[:, :kend]
        s = jnp.einsum('bqhjd,bkhjd->bhjqk', qb, kb).astype(jnp.float32) * scale
        dist = (q0 + jnp.arange(Q_BLOCK, dtype=jnp.float32))[:, None] - jnp.arange(kend, dtype=jnp.float32)[None, :]
        s = s - slopes[None, :, None, None, None] * dist
        s = jnp.where(dist >= 0, s, -jnp.inf)
        p = jax.nn.softmax(s, axis=-1)
        a = p[:, :, 0] - lam * p[:, :, 1]
        outs.append(jnp.einsum('bhqk,bkhe->bqhe', a.astype(vb.dtype), vb))
    return jnp.concatenate(outs, axis=1)


def setup_inputs(seed: int = 0) -> dict:
    key = jax.random.key(seed)
    ks = jax.random.split(key, 16)
    f = jnp.float32

    def nrm(k, shape, scale):
        return jax.random.normal(k, shape, f) * scale

    def gain(k, shape):
        return 1.0 + 0.02 * jax.random.normal(k, shape, f)

    return {
        "x": jax.random.normal(ks[0], (BATCH, SEQ, D_MODEL), f),
        "attn_norm": gain(ks[1], (DEPTH, D_MODEL)),
        "w_in": nrm(ks[2], (DEPTH, D_MODEL, IN_WIDTH), D_MODEL ** -0.5),
        "ret_norm": gain(ks[3], (DEPTH, RET_V_DIM)),
        "lambda_q1": nrm(ks[4], (DEPTH, DIFF_HEAD_DIM), 0.1),
        "lambda_k1": nrm(ks[5], (DEPTH, DIFF_HEAD_DIM), 0.1),
        "lambda_q2": nrm(ks[6], (DEPTH, DIFF_HEAD_DIM), 0.1),
        "lambda_k2": nrm(ks[7], (DEPTH, DIFF_HEAD_DIM), 0.1),
        "diff_norm": gain(ks[8], (DEPTH, DIFF_V_DIM)),
        "w_out": nrm(ks[9], (DEPTH, MIX_WIDTH, D_MODEL), MIX_WIDTH ** -0.5),
        "ffn_norm": gain(ks[10], (DEPTH, D_MODEL)),
        "w_gate": nrm(ks[11], (DEPTH, D_MODEL, FFN_HIDDEN), D_MODEL ** -0.5),
        "w_up": nrm(ks[12], (DEPTH, D_MODEL, FFN_HIDDEN), D_MODEL ** -0.5),
        "w_down": nrm(ks[13], (DEPTH, FFN_HIDDEN, D_MODEL), FFN_HIDDEN ** -0.5),
        "final_norm": gain(ks[14], (D_MODEL,)),
    }


def reference(x, attn_norm, w_in, ret_norm, lambda_q1, lambda_k1, lambda_q2, lambda_k2,
              diff_norm, w_out, ffn_norm, w_gate, w_up, w_down, final_norm):
    B, S, _ = x.shape
    offsets = np.cumsum(IN_SPLITS)[:-1].tolist()
    h = x
    for l in range(DEPTH):
        u = rmsnorm(h, attn_norm[l])
        proj = u @ w_in[l]
        rq, rk, rv, rg, dq, dk, dv = jnp.split(proj, offsets, axis=-1)

        ret = retention_chunkwise(rq.reshape(B, S, RET_HEADS, RET_QK_DIM),
                                  rk.reshape(B, S, RET_HEADS, RET_QK_DIM),
                                  rv.reshape(B, S, RET_HEADS, RET_V_DIM))
        ret = rmsnorm(ret, ret_norm[l]).reshape(B, S, RET_V_W)
        ret = jax.nn.silu(rg) * ret

        lam_init = 0.8 - 0.6 * math.exp(-0.3 * l)
        lam = (jnp.exp(jnp.sum(lambda_q1[l].astype(jnp.float32) * lambda_k1[l].astype(jnp.float32)))
               - jnp.exp(jnp.sum(lambda_q2[l].astype(jnp.float32) * lambda_k2[l].astype(jnp.float32)))
               + lam_init)
        dif = diff_attention(dq.reshape(B, S, DIFF_HEADS, 2, DIFF_HEAD_DIM),
                             dk.reshape(B, S, DIFF_HEADS, 2, DIFF_HEAD_DIM),
                             dv.reshape(B, S, DIFF_HEADS, DIFF_V_DIM), lam)
        dif = (rmsnorm(dif, diff_norm[l]) * (1.0 - lam_init)).reshape(B, S, DIFF_V_W)

        mix = jnp.concatenate([ret, dif], axis=-1)
        h = h + mix @ w_out[l]

        u = rmsnorm(h, ffn_norm[l])
        h = h + (jax.nn.silu(u @ w_gate[l]) * (u @ w_up[l])) @ w_down[l]
    return rmsnorm(h, final_norm)
```

```python
import math
import os
from contextlib import ExitStack

import numpy as np
import concourse.bass as bass
import concourse.mybir as mybir
from concourse.bass_utils import run_bass_kernel_spmd

F32 = mybir.dt.float32
BF16 = mybir.dt.bfloat16
AF = mybir.ActivationFunctionType
ALU = mybir.AluOpType
AX = mybir.AxisListType

D = 1024
S = 2048
DEPTH = 2
BATCH = 32
NCORES = 8
FF = 2816
NJ = FF // 128
INW = 3072
EPS = 1e-6
OFF_RQ, OFF_RK, OFF_RV, OFF_RG, OFF_DQ, OFF_DK, OFF_DV = 0, 256, 512, 1024, 1536, 2048, 2560
NTB = S // 128
NT5 = S // 512


class R:
    __slots__ = ("w", "rs")

    def __init__(self):
        self.w = None
        self.rs = {}


class Eng:
    def __init__(self, name, handle):
        self.name = name
        self.h = handle
        self.sem = None
        self.count = 0
        self.known = {}


class DSem:
    def __init__(self, sem):
        self.sem = sem
        self.count = 0


class Tracker:
    def __init__(self, nc, es):
        self.nc = nc
        self.es = es
        self.nsem = 0
        self.engs = {
            "pe": Eng("pe", nc.tensor),
            "act": Eng("act", nc.scalar),
            "dve": Eng("dve", nc.vector),
            "pool": Eng("pool", nc.gpsimd),
            "sp": Eng("sp", nc.sync),
        }
        self.dsems = []
        self.epoch()

    def new_sem(self):
        self.nsem += 1
        return self.es.enter_context(self.nc.semaphore("ts%d" % self.nsem))

    def epoch(self):
        for e in self.engs.values():
            if e.name == "sp":
                continue
            e.sem = self.new_sem()
            e.count = 0

    def dsem(self):
        d = DSem(self.new_sem())
        self.dsems.append(d)
        return d

    def _waits(self, e, r, w):
        waits = {}

        def need(tok):
            if tok is None:
                return
            sem, val = tok
            if e.name == "pe" and sem is e.sem:
                return
            if e.known.get(sem, 0) >= val:
                return
            if waits.get(sem, (None, 0))[1] < val:
                waits[sem] = (sem, val)

        for x in r:
            need(x.w)
        for x in w:
            need(x.w)
            for t in x.rs.values():
                need(t)
        for sem, val in waits.values():
            e.known[sem] = val
            e.h.wait_ge(sem, val)

    @staticmethod
    def _mark(tok, r, w):
        for x in r:
            old = x.rs.get(tok[0])
            if old is None or old[1] < tok[1]:
                x.rs[tok[0]] = tok
        for x in w:
            x.w = tok
            x.rs = {}

    def op(self, eng, fn, r=(), w=()):
        e = self.engs[eng]
        self._waits(e, r, w)
        ins = fn(e.h)
        e.count += 1
        tok = (e.sem, e.count)
        ins.then_inc(e.sem, 1)
        self._mark(tok, r, w)
        return tok

    def dma(self, out, in_, r, w, ds):
        e = self.engs["sp"]
        self._waits(e, r, w)
        ins = e.h.dma_start(out=out, in_=in_)
        ds.count += 16
        tok = (ds.sem, ds.count)
        ins.then_inc(ds.sem, 16)
        self._mark(tok, r, w)
        return tok

    def barrier(self):
        toks = []
        for e in self.engs.values():
            if e.name != "sp" and e.count > 0:
                toks.append((e.sem, e.count))
        for d in self.dsems:
            if d.count > 0:
                toks.append((d.sem, d.count))
        for e in self.engs.values():
            for sem, val in toks:
                if sem is e.sem:
                    continue
                if e.known.get(sem, 0) >= val:
                    continue
                e.known[sem] = val
                e.h.wait_ge(sem, val)

    def finish(self, toks):
        e = self.engs["sp"]
        for sem, val in toks:
            if e.known.get(sem, 0) >= val:
                continue
            e.known[sem] = val
            e.h.wait_ge(sem, val)


def ret_gamma(h):
    return 1.0 - 2.0 ** (-5.0 - h)


def alibi_slope(h):
    return 2.0 ** (-8.0 * (h + 1) / 4.0)


def build_program(nseq, depth):
    nc = bass.Bass("TRN2", target_bir_lowering=False)
    es = ExitStack()

    def dram(name, shape, kind="ExternalInput", dt=F32):
        return nc.dram_tensor(name, list(shape), dt, kind=kind).ap()

    x = dram("x", [nseq, S, D])
    y = dram("y", [nseq, S, D], kind="ExternalOutput")
    w_in = dram("w_in", [depth, D, INW])
    w_out = dram("w_out", [depth, D, D])
    w_gate = dram("w_gate", [depth, D, FF])
    w_up = dram("w_up", [depth, D, FF])
    w_down = dram("w_down", [depth, FF, D])
    attn_bc = dram("attn_bc", [depth, 128, D])
    ffn_bc = dram("ffn_bc", [depth, 128, D])
    final_bc = dram("final_bc", [128, D])
    retn_col = dram("retn_col", [128, depth])
    difn_col = dram("difn_col", [128, depth])
    lam_in = dram("lam_in", [128, depth * 4 * 64])
    c_ident = dram("c_ident", [128, 128])
    c_dm = dram("c_dm", [128, 896])
    c_df = dram("c_df", [128, 512])
    c_dt = dram("c_dt", [128, 16])
    c_qi = dram("c_qi", [128, 512])

    tk = Tracker(nc, es)

    def sb(name, shape, dt):
        return es.enter_context(nc.sbuf_tensor(name, list(shape), dt))

    ident = sb("ident", [128, 128], F32)
    ones_f = sb("ones_f", [128, 128], F32)
    ones_b = sb("ones_b", [128, 128], BF16)
    dmc = sb("dmc", [128, 896], F32)
    maskm = sb("maskm", [128, 896], F32)
    dfull = sb("dfull", [128, 512], F32)
    cdt = sb("cdt", [128, 16], F32)
    cqi = sb("cqi", [128, 512], F32)
    retn = sb("retn", [128, depth], F32)
    difn = sb("difn", [128, depth], F32)
    lamt = sb("lamt", [128, depth * 4 * 64], F32)
    lamw = sb("lamw", [128, 2 * 64], F32)
    lams = sb("lams", [128, 8], F32)
    neglam = sb("neglam", [128, depth], F32)
    r_const = R()
    r_lam = R()
    cds = tk.dsem()
    tk.dma(ident[:], c_ident[:, :], [], [r_const], cds)
    tk.dma(dmc[:], c_dm[:, :], [], [r_const], cds)
    tk.dma(dfull[:], c_df[:, :], [], [r_const], cds)
    tk.dma(cdt[:], c_dt[:, :], [], [r_const], cds)
    tk.dma(cqi[:], c_qi[:, :], [], [r_const], cds)
    tk.dma(retn[:], retn_col[:, :], [], [r_const], cds)
    tk.dma(difn[:], difn_col[:, :], [], [r_const], cds)
    tk.dma(lamt[:], lam_in[:, :], [], [r_const], cds)
    r_const.w = (cds.sem, cds.count)
    tk.op("dve", lambda v: v.memset(ones_f[:], 1.0), [], [r_const])
    tk.op("dve", lambda v: v.memset(ones_b[:], 1.0), [], [r_const])
    tk.op("dve", lambda v: v.tensor_single_scalar(out=maskm[:], in_=dmc[:], scalar=0.0, op=ALU.is_ge),
          [r_const], [r_const])
    tk.op("dve", lambda v: v.tensor_scalar_max(out=dmc[:], in0=dmc[:], scalar1=0.0), [r_const], [r_const])
    for l in range(depth):
        base = l * 256
        tk.op("dve", lambda v, b=base: v.tensor_tensor(out=lamw[:, 0:64], in0=lamt[:, b:b + 64],
                                                       in1=lamt[:, b + 64:b + 128], op=ALU.mult),
              [r_const], [r_lam])
        tk.op("dve", lambda v, b=base: v.tensor_tensor(out=lamw[:, 64:128], in0=lamt[:, b + 128:b + 192],
                                                       in1=lamt[:, b + 192:b + 256], op=ALU.mult),
              [r_const], [r_lam])
        tk.op("dve", lambda v: v.reduce_sum(out=lams[:, 0:1], in_=lamw[:, 0:64], axis=AX.X), [r_lam], [r_lam])
        tk.op("dve", lambda v: v.reduce_sum(out=lams[:, 1:2], in_=lamw[:, 64:128], axis=AX.X), [r_lam], [r_lam])
        tk.op("act", lambda a: a.activation(out=lams[:, 2:4], in_=lams[:, 0:2], func=AF.Exp), [r_lam], [r_lam])
        lam_init = 0.8 - 0.6 * math.exp(-0.3 * l)
        tk.op("dve", lambda v, li=lam_init: v.tensor_scalar(out=lams[:, 4:5], in0=lams[:, 3:4], scalar1=-li,
                                                            scalar2=None, op0=ALU.add), [r_lam], [r_lam])
        tk.op("dve", lambda v, l=l: v.tensor_tensor(out=neglam[:, l:l + 1], in0=lams[:, 4:5], in1=lams[:, 2:3],
                                                    op=ALU.subtract), [r_lam], [r_const])
        tk.op("dve", lambda v, l=l, li=lam_init: v.tensor_scalar(out=difn[:, l:l + 1], in0=difn[:, l:l + 1],
                                                                 scalar1=1.0 - li, scalar2=None, op0=ALU.mult),
              [r_const], [r_const])

    banks = [es.enter_context(nc.psum_tensor("bank%d" % i, [128, 512], F32)) for i in range(8)]
    rbank = [R() for _ in range(8)]

    NSTG, NWS = 2, 4
    stg = [sb("stg%d" % i, [128, 1024], F32) for i in range(NSTG)]
    r_stg = [R() for _ in range(NSTG)]
    d_stg = [tk.dsem() for _ in range(NSTG)]
    wsl = [sb("wsl%d" % i, [128, 1024], BF16) for i in range(NWS)]
    r_wsl = [R() for _ in range(NWS)]
    ctr = {"stg": 0, "wsl": 0, "bank": 0, "hb": 0, "ev": 0}

    def stage(src_ap, view3=None):
        i = ctr["stg"] % NSTG
        ctr["stg"] += 1
        dst = stg[i][:] if view3 is None else stg[i][:].rearrange("p (a b) -> p a b", a=view3[0], b=view3[1])
        tk.dma(dst, src_ap, [], [r_stg[i]], d_stg[i])
        return stg[i], r_stg[i]

    def cast(eng, out_ap, in_ap, r, w):
        if eng == "act":
            tk.op("act", lambda a: a.activation(out=out_ap, in_=in_ap, func=AF.Copy), r, w)
        else:
            tk.op(eng, lambda g: g.tensor_copy(out=out_ap, in_=in_ap), r, w)

    NSLAB = 68
    wscr = nc.dram_tensor("wscr", [depth * NSLAB, 128, 1024], BF16).ap()
    woscr = nc.dram_tensor("woscr", [depth, 128, 8 * D], BF16).ap()
    wdscr = nc.dram_tensor("wdscr", [depth, 128, NJ * D], BF16).ap()
    d_wsl = [tk.dsem() for _ in range(NWS)]
    d_wst = [tk.dsem() for _ in range(NWS)]
    d_big = tk.dsem()
    cur = {"seq": 0}

    def load_slab(src_ap, eng="pool", key=None):
        j = ctr["wsl"] % NWS
        ctr["wsl"] += 1
        if cur["seq"] == 0:
            st, rst = stage(src_ap, (8, 128))
            cast(eng, wsl[j][:], st[:], [rst], [r_wsl[j]])
            tk.dma(wscr[key], wsl[j][:], [r_wsl[j]], [], d_wst[j])
        else:
            tk.dma(wsl[j][:], wscr[key], [], [r_wsl[j]], d_wsl[j])
        return wsl[j][:].rearrange("p (a b) -> p a b", a=8, b=128), r_wsl[j]

    def wcols(w_ap_l, c0):
        return w_ap_l.rearrange("(kc p) n -> p kc n", p=128)[:, :, c0:c0 + 128]

    def next_bank(lo=0, hi=8):
        i = lo + ctr["bank"] % (hi - lo)
        ctr["bank"] += 1
        return i

    NHB = 3
    hb = [sb("hb%d" % i, [128, D], F32) for i in range(NHB)]
    r_hb = [R() for _ in range(NHB)]
    d_hb = [tk.dsem() for _ in range(NHB)]
    d_hst = [tk.dsem() for _ in range(NHB)]
    NUN = 3
    un = [sb("un%d" % i, [128, D], F32) for i in range(NUN)]
    r_un = [R() for _ in range(NUN)]
    junk = sb("junk", [128, D], BF16)
    r_junk = R()
    gbc = sb("gbc", [128, D], F32)
    r_gbc = R()
    d_gbc = tk.dsem()
    stat = sb("stat", [128, 3 * NTB], F32)
    r_stat = [R() for _ in range(NTB)]

    def blk(ap_seq, tb):
        return ap_seq.rearrange("(tb p) d -> p tb d", p=128)[:, tb, :]

    out_tokens = []

    def norm_stats(i, tb):
        tk.op("act", lambda a: a.activation(out=junk[:], in_=hb[i][:], func=AF.Square,
                                            accum_out=stat[:, tb:tb + 1]),
              [r_hb[i]], [r_junk, r_stat[tb]])
        tk.op("act", lambda a: a.activation(out=stat[:, NTB + tb:NTB + tb + 1], in_=stat[:, tb:tb + 1],
                                            func=AF.Sqrt, scale=1.0 / D, bias=EPS),
              [r_stat[tb]], [r_stat[tb]])
        tk.op("dve", lambda v: v.reciprocal(out=stat[:, 2 * NTB + tb:2 * NTB + tb + 1],
                                            in_=stat[:, NTB + tb:NTB + tb + 1]),
              [r_stat[tb]], [r_stat[tb]])

    for seq in range(nseq):
        cur["seq"] = seq
        for l in range(depth):
            skey = {"i": l * NSLAB}

            def nkey():
                skey["i"] += 1
                return skey["i"] - 1
            tk.barrier()
            tk.epoch()
            hsrc = x[seq] if l == 0 else y[seq]
            with ExitStack() as sl:
                uT = sl.enter_context(nc.sbuf_tensor("uT_%d_%d" % (seq, l), [128, 8, S], BF16))
                r_uT = [R() for _ in range(NTB)]

                def norm_phase(src, g_dram):
                    tk.dma(gbc[:], g_dram, [], [r_gbc], d_gbc)

                    def stage_a(tb):
                        i = ctr["hb"] % NHB
                        ctr["hb"] += 1
                        tk.dma(hb[i][:], blk(src, tb), [], [r_hb[i]], d_hb[i])
                        norm_stats(i, tb)
                        u = tb % NUN
                        tk.op("dve", lambda v: v.scalar_tensor_tensor(
                            out=un[u][:], in0=hb[i][:], scalar=stat[:, 2 * NTB + tb:2 * NTB + tb + 1],
                            in1=gbc[:], op0=ALU.mult, op1=ALU.mult),
                            [r_hb[i], r_stat[tb], r_gbc], [r_un[u]])

                    def stage_b(tb):
                        u = tb % NUN
                        for half in range(2):
                            b = next_bank()

                            def tr(t, b=b, half=half):
                                ins = None
                                for q in range(4):
                                    kc = half * 4 + q
                                    ins = t.transpose(out=banks[b][:, q * 128:(q + 1) * 128],
                                                      in_=un[u][:, kc * 128:(kc + 1) * 128], identity=ident[:])
                                return ins
                            tk.op("pe", tr, [r_un[u], r_const], [rbank[b]])
                            src_ps = banks[b][:].rearrange("p (a b) -> p a b", a=4, b=128)
                            dst = uT[:, half * 4:half * 4 + 4, tb * 128:(tb + 1) * 128]
                            if half == 0:
                                tk.op("act", lambda a, s=src_ps, d=dst: a.activation(out=d, in_=s, func=AF.Copy),
                                      [rbank[b]], [r_uT[tb]])
                            else:
                                tk.op("dve", lambda v, s=src_ps, d=dst: v.tensor_copy(out=d, in_=s),
                                      [rbank[b]], [r_uT[tb]])

                    stage_a(0)
                    stage_a(1)
                    for tb in range(NTB):
                        if tb + 2 < NTB:
                            stage_a(tb + 2)
                        stage_b(tb)

                norm_phase(hsrc, attn_bc[l])

                with ExitStack() as att:
                    def asb(name, shape, dt):
                        return att.enter_context(nc.sbuf_tensor("%s_%d_%d" % (name, seq, l), list(shape), dt))
                    mixT = asb("mixT", [128, 8, S], BF16)
                    r_mix = [R() for _ in range(NTB)]
                    QT = asb("QT", [128, S], BF16)
                    QS = asb("QS", [128, S], BF16)
                    r_qs = [R() for _ in range(NT5)]
                    Bt = asb("Bt", [128, 512], F32)
                    lgcol = asb("lgcol", [128, 1], F32)
                    tabT = [asb("tabT%d" % s_, [128, 16], F32) for s_ in range(2)]
                    r_tab = R()
                    KT = asb("KT", [128, S], BF16)
                    Vt = asb("Vt", [128, NTB, 2, 128], BF16)
                    SG = asb("SG", [128, 2, S], BF16)
                    r_q = [R() for _ in range(NT5)]
                    r_k = [R() for _ in range(NT5)]
                    r_v = [R() for _ in range(NT5)]
                    r_sg = [R() for _ in range(NT5)]
                    Mt = [asb("Mt%d" % s_, [128, 896], F32) for s_ in range(2)]
                    Ft = [asb("Ft%d" % s_, [128, 512], F32) for s_ in range(1)]
                    r_dec = [R(), R()]
                    Eb = [asb("Eb%d" % i, [128, 512], F32) for i in range(4)]
                    r_E = [R() for _ in range(4)]
                    Pb = [asb("Pb%d" % i, [128, 512], BF16) for i in range(6)]
                    r_P = [R() for _ in range(6)]
                    ep = [asb("ep%d" % i, [128, 512], F32) for i in range(6)]
                    r_ep = [R() for _ in range(6)]
                    ectr = {"E": 0, "P": 0}

                    def proj_fm(slab, rsl, t5, evac):
                        b = next_bank(0, 4)

                        def mm(t):
                            ins = None
                            for kc in range(8):
                                ins = t.matmul(banks[b][:, :], slab[:, kc, :], uT[:, kc, t5 * 512:(t5 + 1) * 512],
                                               start=(kc == 0), stop=(kc == 7))
                            return ins
                        tk.op("pe", mm, [rsl] + r_uT[t5 * 4:t5 * 4 + 4], [rbank[b]])
                        evac(b)

                    def evac_copy(dst_ap, rdst):
                        def f(b):
                            k = ctr["ev"]
                            ctr["ev"] += 1
                            if k % 2 == 0:
                                tk.op("act", lambda a: a.activation(out=dst_ap, in_=banks[b][:, :], func=AF.Copy),
                                      [rbank[b]], [rdst])
                            else:
                                tk.op("dve", lambda v: v.tensor_copy(out=dst_ap, in_=banks[b][:, :]),
                                      [rbank[b]], [rdst])
                        return f

                    def proj_v(slab, rsl, hh):
                        for g4 in range(NT5):
                            b = next_bank(0, 4)

                            def mm(t):
                                ins = None
                                for q in range(4):
                                    tb = g4 * 4 + q
                                    for kc in range(8):
                                        ins = t.matmul(banks[b][:, q * 128:(q + 1) * 128],
                                                       uT[:, kc, tb * 128:(tb + 1) * 128], slab[:, kc, :],
                                                       start=(kc == 0), stop=(kc == 7))
                                return ins
                            tk.op("pe", mm, [rsl] + r_uT[g4 * 4:g4 * 4 + 4], [rbank[b]])
                            src_ps = banks[b][:].rearrange("p (a b) -> p a b", a=4, b=128)
                            dst = Vt[:, g4 * 4:g4 * 4 + 4, hh, :]
                            k = ctr["ev"]
                            ctr["ev"] += 1
                            if k % 2 == 0:
                                tk.op("act", lambda a, s=src_ps, d=dst: a.activation(out=d, in_=s, func=AF.Copy),
                                      [rbank[b]], [r_v[g4]])
                            else:
                                tk.op("dve", lambda v, s=src_ps, d=dst: v.tensor_copy(out=d, in_=s),
                                      [rbank[b]], [r_v[g4]])

                    units = [("ret", 0), ("ret", 1)] + [("diff", h_) for h_ in range(4)]
                    for kind, ui in units:
                        is_ret = kind == "ret"
                        new_path = is_ret or ui > 0
                        RET_NEW = os.environ.get('KDBG_RET_NEW', '1') == '1'
                        DIFF_NEW = os.environ.get('KDBG_DIFF_NEW', '1') == '1'
                        for s_ in range(2 if is_ret else 1):
                            if is_ret:
                                lg = math.log(ret_gamma(2 * ui + s_))
                                bias = math.log(0.125)
                            else:
                                lg = -alibi_slope(ui)
                                bias = 0.0
                            if is_ret or ui == 0 or not DIFF_NEW:
                                tk.op("act", lambda a, s_=s_, lg=lg, bias=bias: a.activation(
                                    out=Mt[s_][:], in_=dmc[:], func=AF.Exp, scale=lg, bias=bias),
                                    [r_const], [r_dec[s_]])
                                tk.op("pool", lambda g, s_=s_: g.tensor_tensor(out=Mt[s_][:], in0=Mt[s_][:],
                                                                               in1=maskm[:], op=ALU.mult),
                                      [r_const, r_dec[s_]], [r_dec[s_]])
                            if (not is_ret) and (ui == 0 or not DIFF_NEW):
                                tk.op("act", lambda a, s_=s_, lg=lg, bias=bias: a.activation(
                                    out=Ft[s_][:], in_=dfull[:], func=AF.Exp, scale=lg, bias=bias),
                                    [r_const], [r_dec[s_]])
                            if is_ret:
                                tk.op("act", lambda a, s_=s_, lg=lg, bias=bias: a.activation(
                                    out=tabT[s_][:], in_=cdt[:], func=AF.Exp, scale=-lg, bias=bias),
                                    [r_const], [r_tab])
                                tk.op("dve", lambda v, s_=s_, lg=lg: v.memset(lgcol[64 * s_:64 * s_ + 64, :], lg),
                                      [], [r_tab])
                        if is_ret:
                            tk.op("act", lambda a: a.activation(out=Bt[:], in_=cqi[:], func=AF.Exp,
                                                                scale=lgcol[:, 0:1]),
                                  [r_const, r_tab], [r_tab])
                        elif ui > 0:
                            tk.op("dve", lambda v: v.tensor_scalar(out=tabT[0][:], in0=cdt[:],
                                                                   scalar1=float(alibi_slope(ui)), scalar2=None,
                                                                   op0=ALU.mult),
                                  [r_const], [r_tab])
                        if is_ret:
                            qc, kc_, vc, gc = OFF_RQ + 128 * ui, OFF_RK + 128 * ui, OFF_RV + 256 * ui, OFF_RG + 256 * ui
                        else:
                            qc, kc_, vc = OFF_DQ + 128 * ui, OFF_DK + 128 * ui, OFF_DV + 128 * ui
                        slab, rsl = load_slab(wcols(w_in[l], qc), key=nkey())
                        for t5 in range(NT5):
                            if is_ret:
                                def evq(b, t5=t5):
                                    tk.op("act", lambda a: a.activation(out=QT[:, t5 * 512:(t5 + 1) * 512],
                                                                        in_=banks[b][:, :], func=AF.Copy),
                                          [rbank[b]], [r_q[t5]])
                                    tk.op("dve", lambda v: v.tensor_tensor(out=QS[:, t5 * 512:(t5 + 1) * 512],
                                                                           in0=banks[b][:, :], in1=Bt[:], op=ALU.mult),
                                          [r_tab], [rbank[b], r_qs[t5]])
                                proj_fm(slab, rsl, t5, evq)
                            else:
                                proj_fm(slab, rsl, t5, evac_copy(QT[:, t5 * 512:(t5 + 1) * 512], r_q[t5]))
                        slab, rsl = load_slab(wcols(w_in[l], kc_), key=nkey())
                        for t5 in range(NT5):
                            proj_fm(slab, rsl, t5, evac_copy(KT[:, t5 * 512:(t5 + 1) * 512], r_k[t5]))
                        for hh in range(2 if is_ret else 1):
                            slab, rsl = load_slab(wcols(w_in[l], vc + 128 * hh), key=nkey())
                            proj_v(slab, rsl, hh)
                        if is_ret:
                            for hh in range(2):
                                slab, rsl = load_slab(wcols(w_in[l], gc + 128 * hh), key=nkey())
                                for t5 in range(NT5):
                                    def ev(b, hh=hh, t5=t5):
                                        tk.op("act", lambda a: a.activation(
                                            out=SG[:, hh, t5 * 512:(t5 + 1) * 512], in_=banks[b][:, :], func=AF.Silu),
                                            [rbank[b]], [r_sg[t5]])
                                    proj_fm(slab, rsl, t5, ev)

                        OB = [4, 5]
                        ZB = [6, 7]
                        for t5 in range(NT5):
                            nkb = 4 * (t5 + 1)
                            ring = {"i": 0}

                            def issue_S(b_):
                                sb_ = [(ring["i"] % 2) * 2, (ring["i"] % 2) * 2 + 1]
                                ring["i"] += 1
                                offd = (512 * t5 - 128 * b_) >= 128
                                Qsrc, rq_ = (QS, r_qs[t5]) if (is_ret and offd and RET_NEW) else (QT, r_q[t5])
                                for s_ in range(2):
                                    bk = sb_[s_]
                                    tk.op("pe", lambda t, bk=bk, s_=s_: t.matmul(
                                        banks[bk][:, :], KT[64 * s_:64 * s_ + 64, b_ * 128:(b_ + 1) * 128],
                                        Qsrc[64 * s_:64 * s_ + 64, t5 * 512:(t5 + 1) * 512], start=True, stop=True),
                                        [r_k[b_ // 4], rq_], [rbank[bk]])
                                return sb_

                            def make_P(b_, sb_):
                                delta = 512 * t5 - 128 * b_
                                offd = delta >= 128
                                m = delta // 128 + 3
                                ps = []
                                for s_ in range(2):
                                    ds_ = s_ if is_ret else 0
                                    pi = ectr["P"] % 6
                                    ectr["P"] += 1
                                    bk = sb_[s_]
                                    if is_ret:
                                        if offd and not RET_NEW:
                                            c = ret_gamma(2 * ui + s_) ** delta
                                            tk.op("dve", lambda v, bk=bk, pi=pi, c=c: v.scalar_tensor_tensor(
                                                out=Pb[pi][:], in0=banks[bk][:, :], scalar=float(c), in1=Ft[0][:, :],
                                                op0=ALU.mult, op1=ALU.mult),
                                                [rbank[bk], r_dec[0]], [r_P[pi]])
                                        elif offd:
                                            tk.op("act", lambda a, bk=bk, pi=pi, s_=s_: a.activation(
                                                out=Pb[pi][:], in_=banks[bk][:, :], func=AF.Copy,
                                                scale=tabT[s_][:, m:m + 1]),
                                                [rbank[bk], r_tab], [r_P[pi]])
                                        else:
                                            tile_ap = Mt[ds_][:, 384 + delta:384 + delta + 512]
                                            tk.op("dve", lambda v, bk=bk, pi=pi, tile_ap=tile_ap: v.tensor_tensor(
                                                out=Pb[pi][:], in0=banks[bk][:, :], in1=tile_ap, op=ALU.mult),
                                                [rbank[bk], r_dec[ds_]], [r_P[pi]])
                                    elif ui == 0 or not DIFF_NEW:
                                        tile_ap = Ft[0][:, :] if offd else Mt[0][:, 384 + delta:384 + delta + 512]
                                        cb = -alibi_slope(ui) * delta if offd else 0.0
                                        ei = ectr["E"] % 4
                                        ectr["E"] += 1
                                        tk.op("act", lambda a, bk=bk, ei=ei, cb=cb: a.activation(
                                            out=Eb[ei][:], in_=banks[bk][:, :], func=AF.Exp, scale=0.125, bias=float(cb)),
                                            [rbank[bk]], [r_E[ei]])
                                        tk.op("dve", lambda v, ei=ei, pi=pi, tile_ap=tile_ap: v.tensor_tensor(
                                            out=Pb[pi][:], in0=Eb[ei][:], in1=tile_ap, op=ALU.mult),
                                            [r_E[ei], r_dec[0]], [r_P[pi]])
                                    else:
                                        if offd:
                                            tk.op("act", lambda a, bk=bk, pi=pi: a.activation(
                                                out=Pb[pi][:], in_=banks[bk][:, :], func=AF.Exp, scale=0.125,
                                                bias=tabT[0][:, m:m + 1]),
                                                [rbank[bk], r_tab], [r_P[pi]])
                                        else:
                                            ei = ectr["E"] % 4
                                            ectr["E"] += 1
                                            tk.op("act", lambda a, bk=bk, ei=ei: a.activation(
                                                out=Eb[ei][:], in_=banks[bk][:, :], func=AF.Exp, scale=0.125,
                                                bias=tabT[0][:, m:m + 1]),
                                                [rbank[bk], r_tab], [r_E[ei]])
                                            mk = maskm[:, 384 + delta:384 + delta + 512]
                                            tk.op("dve", lambda v, ei=ei, pi=pi, mk=mk: v.tensor_tensor(
                                                out=Pb[pi][:], in0=Eb[ei][:], in1=mk, op=ALU.mult),
                                                [r_E[ei], r_const], [r_P[pi]])
                                    ps.append(pi)
                                return ps

                            def issue_PV(b_, ps):
                                first, last = b_ == 0, b_ == nkb - 1
                                for s_ in range(2):
                                    pi = ps[s_]
                                    vh = s_ if is_ret else 0
                                    if not is_ret:
                                        tk.op("pe", lambda t, s_=s_, pi=pi: t.matmul(
                                            banks[ZB[s_]][:, :], ones_b[:, :], Pb[pi][:, :], start=first, stop=last),
                                            [r_P[pi], r_const], [rbank[ZB[s_]]])
                                    tk.op("pe", lambda t, s_=s_, pi=pi, vh=vh: t.matmul(
                                        banks[OB[s_]][:, :], Vt[:, b_, vh, :], Pb[pi][:, :], start=first, stop=last),
                                        [r_P[pi], r_v[b_ // 4]], [rbank[OB[s_]]])

                            prev = None
                            for b_ in range(nkb):
                                sb_ = issue_S(b_)
                                if prev is not None:
                                    issue_PV(*prev)
                                ps = make_P(b_, sb_)
                                prev = (b_, ps)
                            issue_PV(*prev)

                            qs = slice(t5 * 512, (t5 + 1) * 512)
                            tbs = r_mix[t5 * 4:t5 * 4 + 4]
                            if is_ret:
                                for s_ in range(2):
                                    head = 2 * ui + s_
                                    ob = OB[s_]
                                    e0, e1, e2 = ep[3 * s_], ep[3 * s_ + 1], ep[3 * s_ + 2]
                                    re0, re1, re2 = r_ep[3 * s_], r_ep[3 * s_ + 1], r_ep[3 * s_ + 2]
                                    tk.op("act", lambda a, ob=ob, e0=e0: a.activation(out=e0[:], in_=banks[ob][:, :],
                                                                                      func=AF.Square),
                                          [rbank[ob]], [re0])
                                    zb = ZB[s_]
                                    tk.op("pe", lambda t, zb=zb, e0=e0: t.matmul(banks[zb][:, :], ones_f[:, :], e0[:],
                                                                                 start=True, stop=True),
                                          [re0, r_const], [rbank[zb]])
                                    tk.op("act", lambda a, zb=zb, e1=e1: a.activation(out=e1[:], in_=banks[zb][:, :],
                                                                                      func=AF.Ln, scale=1.0 / 128, bias=EPS),
                                          [rbank[zb]], [re1])
                                    tk.op("act", lambda a, e1=e1: a.activation(out=e1[:], in_=e1[:], func=AF.Exp, scale=-0.5),
                                          [re1], [re1])
                                    tk.op("dve", lambda v, ob=ob, e1=e1, e2=e2: v.scalar_tensor_tensor(
                                        out=e2[:], in0=banks[ob][:, :], scalar=retn[:, l:l + 1], in1=e1[:],
                                        op0=ALU.mult, op1=ALU.mult),
                                        [rbank[ob], re1, r_const], [re2])
                                    tk.op("pool", lambda g, e2=e2, s_=s_, head=head: g.tensor_tensor(
                                        out=mixT[:, head, qs], in0=e2[:], in1=SG[:, s_, qs], op=ALU.mult),
                                        [re2, r_sg[t5]], tbs)
                            else:
                                head = 4 + ui
                                e0, e1, e2, e3, e4, e5 = ep
                                tk.op("act", lambda a: a.activation(out=e0[:], in_=banks[ZB[0]][:, :], func=AF.Ln),
                                      [rbank[ZB[0]]], [r_ep[0]])
                                tk.op("act", lambda a: a.activation(out=e1[:], in_=banks[ZB[1]][:, :], func=AF.Ln),
                                      [rbank[ZB[1]]], [r_ep[1]])
                                tk.op("act", lambda a: a.activation(out=e0[:], in_=e0[:], func=AF.Exp, scale=-1.0),
                                      [r_ep[0]], [r_ep[0]])
                                tk.op("act", lambda a: a.activation(out=e1[:], in_=e1[:], func=AF.Exp, scale=-1.0),
                                      [r_ep[1]], [r_ep[1]])
                                tk.op("dve", lambda v: v.tensor_tensor(out=e2[:], in0=banks[OB[0]][:, :], in1=e0[:],
                                                                       op=ALU.mult),
                                      [rbank[OB[0]], r_ep[0]], [r_ep[2]])
                                tk.op("dve", lambda v: v.tensor_tensor(out=e3[:], in0=banks[OB[1]][:, :], in1=e1[:],
                                                                       op=ALU.mult),
                                      [rbank[OB[1]], r_ep[1]], [r_ep[3]])
                                tk.op("dve", lambda g: g.scalar_tensor_tensor(
                                    out=e4[:], in0=e3[:], scalar=neglam[:, l:l + 1], in1=e2[:],
                                    op0=ALU.mult, op1=ALU.add),
                                    [r_ep[2], r_ep[3], r_const], [r_ep[4]])
                                tk.op("act", lambda a: a.activation(out=e5[:], in_=e4[:], func=AF.Square),
                                      [r_ep[4]], [r_ep[5]])
                                tk.op("pe", lambda t: t.matmul(banks[ZB[0]][:, :], ones_f[:, :], e5[:],
                                                               start=True, stop=True),
                                      [r_ep[5], r_const], [rbank[ZB[0]]])
                                tk.op("act", lambda a: a.activation(out=e0[:], in_=banks[ZB[0]][:, :], func=AF.Ln,
                                                                    scale=1.0 / 128, bias=EPS),
                                      [rbank[ZB[0]]], [r_ep[0]])
                                tk.op("act", lambda a: a.activation(out=e0[:], in_=e0[:], func=AF.Exp, scale=-0.5),
                                      [r_ep[0]], [r_ep[0]])
                                tk.op("dve", lambda v: v.scalar_tensor_tensor(
                                    out=mixT[:, head, qs], in0=e4[:], scalar=difn[:, l:l + 1], in1=e0[:],
                                    op0=ALU.mult, op1=ALU.mult),
                                    [r_ep[4], r_ep[0], r_const], tbs)

                    tk.barrier()
                    with ExitStack() as oo:
                        Wo = oo.enter_context(nc.sbuf_tensor("Wo_%d_%d" % (seq, l), [128, 8, D], BF16))
                        r_wo = R()
                        wo_flat = Wo[:].rearrange("p a b -> p (a b)")
                        if seq == 0:
                            for kc in range(8):
                                st, rst = stage(w_out[l][kc * 128:(kc + 1) * 128, :])
                                cast("act" if kc % 2 == 0 else "dve", Wo[:, kc, :], st[:], [rst], [r_wo])
                            tk.dma(woscr[l], wo_flat, [r_wo], [], d_big)
                        else:
                            tk.dma(wo_flat, woscr[l], [], [r_wo], d_big)
                        for tb in range(NTB):
                            i = ctr["hb"] % NHB
                            ctr["hb"] += 1
                            tk.dma(hb[i][:], blk(hsrc, tb), [], [r_hb[i]], d_hb[i])
                            for nh in range(2):
                                b = next_bank()

                                def mm(t, b=b, nh=nh):
                                    ins = None
                                    for kc in range(8):
                                        ins = t.matmul(banks[b][:, :], mixT[:, kc, tb * 128:(tb + 1) * 128],
                                                       Wo[:, kc, nh * 512:(nh + 1) * 512], start=(kc == 0), stop=(kc == 7))
                                    return ins
                                tk.op("pe", mm, [r_wo, r_mix[tb]], [rbank[b]])
                                tk.op("dve", lambda v, b=b, nh=nh: v.tensor_tensor(
                                    out=hb[i][:, nh * 512:(nh + 1) * 512], in0=hb[i][:, nh * 512:(nh + 1) * 512],
                                    in1=banks[b][:, :], op=ALU.add),
                                    [rbank[b], r_hb[i]], [r_hb[i]])
                            tk.dma(blk(y[seq], tb), hb[i][:], [r_hb[i]], [], d_hst[i])
                        tk.barrier()
                    tk.barrier()

                tk.barrier()
                norm_phase(y[seq], ffn_bc[l])

                with ExitStack() as ff:
                    HT = S // 2
                    gT = ff.enter_context(nc.sbuf_tensor("gT_%d_%d" % (seq, l), [128, NJ, HT], BF16))
                    r_g = [R() for _ in range(NTB)]
                    Wd = ff.enter_context(nc.sbuf_tensor("Wd_%d_%d" % (seq, l), [128, NJ, D], BF16))
                    r_wd = R()
                    sgb = [ff.enter_context(nc.sbuf_tensor("sgb%d_%d_%d" % (i, seq, l), [128, 512], F32))
                           for i in range(2)]
                    r_sgb = [R(), R()]
                    k_sg = 0
                    last = (l == depth - 1)
                    if last:
                        tk.dma(gbc[:], final_bc[:, :], [], [r_gbc], d_gbc)
                    wd_flat = Wd[:].rearrange("p a b -> p (a b)")
                    if seq > 0:
                        tk.dma(wd_flat, wdscr[l], [], [r_wd], d_big)
                    for hf in range(2):
                        if hf == 1 and seq == 0:
                            tk.dma(wdscr[l], wd_flat, [r_wd], [], d_big)
                        for j in range(NJ):
                            slg, rg_ = load_slab(wcols(w_gate[l], j * 128), "act", key=l * NSLAB + 24 + j)
                            slu, ru_ = load_slab(wcols(w_up[l], j * 128), "dve", key=l * NSLAB + 46 + j)
                            if hf == 0 and seq == 0:
                                st, rst = stage(w_down[l][j * 128:(j + 1) * 128, :])
                                cast("pool", Wd[:, j, :], st[:], [rst], [r_wd])
                            for t5 in (2 * hf, 2 * hf + 1):
                                bg = next_bank()
                                bu = next_bank()

                                def mmg(t, bb=bg, slab=slg, t5=t5):
                                    ins = None
                                    for kc in range(8):
                                        ins = t.matmul(banks[bb][:, :], slab[:, kc, :],
                                                       uT[:, kc, t5 * 512:(t5 + 1) * 512],
                                                       start=(kc == 0), stop=(kc == 7))
                                    return ins
                                tk.op("pe", mmg, [rg_] + r_uT[t5 * 4:t5 * 4 + 4], [rbank[bg]])

                                def mmu(t, bb=bu, slab=slu, t5=t5):
                                    ins = None
                                    for kc in range(8):
                                        ins = t.matmul(banks[bb][:, :], slab[:, kc, :],
                                                       uT[:, kc, t5 * 512:(t5 + 1) * 512],
                                                       start=(kc == 0), stop=(kc == 7))
                                    return ins
                                tk.op("pe", mmu, [ru_] + r_uT[t5 * 4:t5 * 4 + 4], [rbank[bu]])
                                si = k_sg % 2
                                k_sg += 1
                                tk.op("act", lambda a, si=si, bg=bg: a.activation(out=sgb[si][:], in_=banks[bg][:, :],
                                                                                  func=AF.Silu),
                                      [rbank[bg]], [r_sgb[si]])
                                lo = (t5 - 2 * hf) * 512
                                tk.op("dve", lambda v, si=si, bu=bu, j=j, lo=lo: v.tensor_tensor(
                                    out=gT[:, j, lo:lo + 512], in0=sgb[si][:], in1=banks[bu][:, :], op=ALU.mult),
                                    [r_sgb[si], rbank[bu]], r_g[t5 * 4:t5 * 4 + 4])
                        for tb in range(8 * hf, 8 * hf + 8):
                            i = ctr["hb"] % NHB
                            ctr["hb"] += 1
                            tk.dma(hb[i][:], blk(y[seq], tb), [], [r_hb[i]], d_hb[i])
                            lo = (tb - 8 * hf) * 128
                            for nh in range(2):
                                b = next_bank()

                                def mm(t, b=b, nh=nh, lo=lo):
                                    ins = None
                                    for j in range(NJ):
                                        ins = t.matmul(banks[b][:, :], gT[:, j, lo:lo + 128],
                                                       Wd[:, j, nh * 512:(nh + 1) * 512],
                                                       start=(j == 0), stop=(j == NJ - 1))
                                    return ins
                                tk.op("pe", mm, [r_wd, r_g[tb]], [rbank[b]])
                                tk.op("dve", lambda v, b=b, nh=nh: v.tensor_tensor(
                                    out=hb[i][:, nh * 512:(nh + 1) * 512], in0=hb[i][:, nh * 512:(nh + 1) * 512],
                                    in1=banks[b][:, :], op=ALU.add),
                                    [rbank[b], r_hb[i]], [r_hb[i]])
                            if last:
                                norm_stats(i, tb)
                                tk.op("dve", lambda v: v.scalar_tensor_tensor(
                                    out=hb[i][:], in0=hb[i][:], scalar=stat[:, 2 * NTB + tb:2 * NTB + tb + 1],
                                    in1=gbc[:], op0=ALU.mult, op1=ALU.mult),
                                    [r_hb[i], r_stat[tb], r_gbc], [r_hb[i]])
                            tok = tk.dma(blk(y[seq], tb), hb[i][:], [r_hb[i]], [], d_hst[i])
                            if last:
                                out_tokens.append(tok)
                    tk.barrier()
                tk.barrier()
    tk.finish(out_tokens)
    tk.barrier()
    es.close()
    return nc


def host_consts():
    p = np.arange(128, dtype=np.float32)[:, None]
    c = np.arange(896, dtype=np.float32)[None, :]
    dm = (c - 384.0 - p).astype(np.float32)
    df = (np.arange(512, dtype=np.float32)[None, :] - p).astype(np.float32)
    ident = np.eye(128, dtype=np.float32)
    dt_ = (p - 128.0 * (np.arange(16, dtype=np.float32)[None, :] - 3.0)).astype(np.float32)
    qi = np.ascontiguousarray(np.broadcast_to(np.arange(512, dtype=np.float32)[None, :], (128, 512)))
    return ident, dm, df, dt_, qi


_CACHE = {}


def run(inputs, nseq, depth, ncores, trace=False):
    key = (nseq, depth)
    if key not in _CACHE:
        _CACHE[key] = build_program(nseq, depth)
    nc = _CACHE[key]
    f = lambda a: np.ascontiguousarray(np.asarray(a, dtype=np.float32))
    ident, dm, df, dt_, qi_ = host_consts()
    bc = lambda a: np.ascontiguousarray(np.broadcast_to(f(a)[:, None, :], (a.shape[0], 128, a.shape[1])))
    lam = np.concatenate([f(inputs["lambda_q1"])[:, None, :], f(inputs["lambda_k1"])[:, None, :],
                          f(inputs["lambda_q2"])[:, None, :], f(inputs["lambda_k2"])[:, None, :]], axis=1)
    lam = lam[:depth].reshape(1, depth * 4 * 64)
    shared = {
        "w_in": f(inputs["w_in"])[:depth], "w_out": f(inputs["w_out"])[:depth],
        "w_gate": f(inputs["w_gate"])[:depth], "w_up": f(inputs["w_up"])[:depth],
        "w_down": f(inputs["w_down"])[:depth],
        "attn_bc": bc(f(inputs["attn_norm"])[:depth]), "ffn_bc": bc(f(inputs["ffn_norm"])[:depth]),
        "final_bc": np.ascontiguousarray(np.broadcast_to(f(inputs["final_norm"])[None, :], (128, D))),
        "retn_col": np.ascontiguousarray(f(inputs["ret_norm"])[:depth].T),
        "difn_col": np.ascontiguousarray(f(inputs["diff_norm"])[:depth].T),
        "lam_in": np.ascontiguousarray(np.broadcast_to(lam, (128, depth * 4 * 64))),
        "c_ident": ident, "c_dm": dm, "c_df": df, "c_dt": dt_, "c_qi": qi_,
    }
    xs = f(inputs["x"])
    in_maps = []
    for c in range(ncores):
        m = dict(shared)
        m["x"] = np.ascontiguousarray(xs[c * nseq:(c + 1) * nseq])
        in_maps.append(m)
    res = run_bass_kernel_spmd(nc, in_maps, core_ids=list(range(ncores)), **({"trace": True} if trace else {}))
    out = np.concatenate([r["y"] for r in res.results], axis=0)
    return out, res


def kernel(x, attn_norm, w_in, ret_norm, lambda_q1, lambda_k1, lambda_q2, lambda_k2,
           diff_norm, w_out, ffn_norm, w_gate, w_up, w_down, final_norm):
    inputs = dict(x=x, attn_norm=attn_norm, w_in=w_in, ret_norm=ret_norm, lambda_q1=lambda_q1,
                  lambda_k1=lambda_k1, lambda_q2=lambda_q2, lambda_k2=lambda_k2, diff_norm=diff_norm,
                  w_out=w_out, ffn_norm=ffn_norm, w_gate=w_gate, w_up=w_up, w_down=w_down,
                  final_norm=final_norm)
    out, _ = run(inputs, BATCH // NCORES, DEPTH, NCORES)
    return out.astype(np.float32)
```

```python
import math
import os
from contextlib import ExitStack

import numpy as np
import concourse.bass as bass
import concourse.mybir as mybir
from concourse.bass_utils import run_bass_kernel_spmd

F32 = mybir.dt.float32
BF16 = mybir.dt.bfloat16
AF = mybir.ActivationFunctionType
ALU = mybir.AluOpType
AX = mybir.AxisListType

D = 1024
S = 2048
DEPTH = 2
BATCH = 32
NCORES = 8
FF = 2816
NJ = FF // 128
INW = 3072
EPS = 1e-6
OFF_RQ, OFF_RK, OFF_RV, OFF_RG, OFF_DQ, OFF_DK, OFF_DV = 0, 256, 512, 1024, 1536, 2048, 2560
NTB = S // 128
NT5 = S // 512


class R:
    __slots__ = ("w", "rs")

    def __init__(self):
        self.w = None
        self.rs = {}


class Eng:
    def __init__(self, name, handle):
        self.name = name
        self.h = handle
        self.sem = None
        self.count = 0
        self.known = {}


class DSem:
    def __init__(self, sem):
        self.sem = sem
        self.count = 0


class Tracker:
    def __init__(self, nc, es):
        self.nc = nc
        self.es = es
        self.nsem = 0
        self.engs = {
            "pe": Eng("pe", nc.tensor),
            "act": Eng("act", nc.scalar),
            "dve": Eng("dve", nc.vector),
            "pool": Eng("pool", nc.gpsimd),
            "sp": Eng("sp", nc.sync),
        }
        self.dsems = []
        self.epoch()

    def new_sem(self):
        self.nsem += 1
        return self.es.enter_context(self.nc.semaphore("ts%d" % self.nsem))

    def epoch(self):
        for e in self.engs.values():
            if e.name == "sp":
                continue
            e.sem = self.new_sem()
            e.count = 0

    def dsem(self):
        d = DSem(self.new_sem())
        self.dsems.append(d)
        return d

    def _waits(self, e, r, w):
        waits = {}

        def need(tok):
            if tok is None:
                return
            sem, val = tok
            if e.name == "pe" and sem is e.sem:
                return
            if e.known.get(sem, 0) >= val:
                return
            if waits.get(sem, (None, 0))[1] < val:
                waits[sem] = (sem, val)

        for x in r:
            need(x.w)
        for x in w:
            need(x.w)
            for t in x.rs.values():
                need(t)
        for sem, val in waits.values():
            e.known[sem] = val
            e.h.wait_ge(sem, val)

    @staticmethod
    def _mark(tok, r, w):
        for x in r:
            old = x.rs.get(tok[0])
            if old is None or old[1] < tok[1]:
                x.rs[tok[0]] = tok
        for x in w:
            x.w = tok
            x.rs = {}

    def op(self, eng, fn, r=(), w=()):
        e = self.engs[eng]
        self._waits(e, r, w)
        ins = fn(e.h)
        e.count += 1
        tok = (e.sem, e.count)
        ins.then_inc(e.sem, 1)
        self._mark(tok, r, w)
        return tok

    def dma(self, out, in_, r, w, ds):
        e = self.engs["sp"]
        self._waits(e, r, w)
        ins = e.h.dma_start(out=out, in_=in_)
        ds.count += 16
        tok = (ds.sem, ds.count)
        ins.then_inc(ds.sem, 16)
        self._mark(tok, r, w)
        return tok

    def barrier(self):
        toks = []
        for e in self.engs.values():
            if e.name != "sp" and e.count > 0:
                toks.append((e.sem, e.count))
        for d in self.dsems:
            if d.count > 0:
                toks.append((d.sem, d.count))
        for e in self.engs.values():
            for sem, val in toks:
                if sem is e.sem:
                    continue
                if e.known.get(sem, 0) >= val:
                    continue
                e.known[sem] = val
                e.h.wait_ge(sem, val)

    def finish(self, toks):
        e = self.engs["sp"]
        for sem, val in toks:
            if e.known.get(sem, 0) >= val:
                continue
            e.known[sem] = val
            e.h.wait_ge(sem, val)


def ret_gamma(h):
    return 1.0 - 2.0 ** (-5.0 - h)


def alibi_slope(h):
    return 2.0 ** (-8.0 * (h + 1) / 4.0)


def build_program(nseq, depth):
    nc = bass.Bass("TRN2", target_bir_lowering=False)
    es = ExitStack()

    def dram(name, shape, kind="ExternalInput", dt=F32):
        return nc.dram_tensor(name, list(shape), dt, kind=kind).ap()

    x = dram("x", [nseq, S, D])
    y = dram("y", [nseq, S, D], kind="ExternalOutput")
    w_in = dram("w_in", [depth, D, INW])
    w_out = dram("w_out", [depth, D, D])
    w_gate = dram("w_gate", [depth, D, FF])
    w_up = dram("w_up", [depth, D, FF])
    w_down = dram("w_down", [depth, FF, D])
    attn_bc = dram("attn_bc", [depth, 128, D])
    ffn_bc = dram("ffn_bc", [depth, 128, D])
    final_bc = dram("final_bc", [128, D])
    retn_col = dram("retn_col", [128, depth])
    difn_col = dram("difn_col", [128, depth])
    lam_in = dram("lam_in", [128, depth * 4 * 64])
    c_ident = dram("c_ident", [128, 128])
    c_dm = dram("c_dm", [128, 896])
    c_df = dram("c_df", [128, 512])
    c_dt = dram("c_dt", [128, 16])
    c_qi = dram("c_qi", [128, 512])

    tk = Tracker(nc, es)

    def sb(name, shape, dt):
        return es.enter_context(nc.sbuf_tensor(name, list(shape), dt))

    ident = sb("ident", [128, 128], F32)
    ones_f = sb("ones_f", [128, 128], F32)
    ones_b = sb("ones_b", [128, 128], BF16)
    dmc = sb("dmc", [128, 896], F32)
    maskm = sb("maskm", [128, 896], F32)
    dfull = sb("dfull", [128, 512], F32)
    cdt = sb("cdt", [128, 16], F32)
    cqi = sb("cqi", [128, 512], F32)
    retn = sb("retn", [128, depth], F32)
    difn = sb("difn", [128, depth], F32)
    lamt = sb("lamt", [128, depth * 4 * 64], F32)
    lamw = sb("lamw", [128, 2 * 64], F32)
    lams = sb("lams", [128, 8], F32)
    neglam = sb("neglam", [128, depth], F32)
    r_const = R()
    r_lam = R()
    cds = tk.dsem()
    tk.dma(ident[:], c_ident[:, :], [], [r_const], cds)
    tk.dma(dmc[:], c_dm[:, :], [], [r_const], cds)
    tk.dma(dfull[:], c_df[:, :], [], [r_const], cds)
    tk.dma(cdt[:], c_dt[:, :], [], [r_const], cds)
    tk.dma(cqi[:], c_qi[:, :], [], [r_const], cds)
    tk.dma(retn[:], retn_col[:, :], [], [r_const], cds)
    tk.dma(difn[:], difn_col[:, :], [], [r_const], cds)
    tk.dma(lamt[:], lam_in[:, :], [], [r_const], cds)
    r_const.w = (cds.sem, cds.count)
    tk.op("dve", lambda v: v.memset(ones_f[:], 1.0), [], [r_const])
    tk.op("dve", lambda v: v.memset(ones_b[:], 1.0), [], [r_const])
    tk.op("dve", lambda v: v.tensor_single_scalar(out=maskm[:], in_=dmc[:], scalar=0.0, op=ALU.is_ge),
          [r_const], [r_const])
    tk.op("dve", lambda v: v.tensor_scalar_max(out=dmc[:], in0=dmc[:], scalar1=0.0), [r_const], [r_const])
    for l in range(depth):
        base = l * 256
        tk.op("dve", lambda v, b=base: v.tensor_tensor(out=lamw[:, 0:64], in0=lamt[:, b:b + 64],
                                                       in1=lamt[:, b + 64:b + 128], op=ALU.mult),
              [r_const], [r_lam])
        tk.op("dve", lambda v, b=base: v.tensor_tensor(out=lamw[:, 64:128], in0=lamt[:, b + 128:b + 192],
                                                       in1=lamt[:, b + 192:b + 256], op=ALU.mult),
              [r_const], [r_lam])
        tk.op("dve", lambda v: v.reduce_sum(out=lams[:, 0:1], in_=lamw[:, 0:64], axis=AX.X), [r_lam], [r_lam])
        tk.op("dve", lambda v: v.reduce_sum(out=lams[:, 1:2], in_=lamw[:, 64:128], axis=AX.X), [r_lam], [r_lam])
        tk.op("act", lambda a: a.activation(out=lams[:, 2:4], in_=lams[:, 0:2], func=AF.Exp), [r_lam], [r_lam])
        lam_init = 0.8 - 0.6 * math.exp(-0.3 * l)
        tk.op("dve", lambda v, li=lam_init: v.tensor_scalar(out=lams[:, 4:5], in0=lams[:, 3:4], scalar1=-li,
                                                            scalar2=None, op0=ALU.add), [r_lam], [r_lam])
        tk.op("dve", lambda v, l=l: v.tensor_tensor(out=neglam[:, l:l + 1], in0=lams[:, 4:5], in1=lams[:, 2:3],
                                                    op=ALU.subtract), [r_lam], [r_const])
        tk.op("dve", lambda v, l=l, li=lam_init: v.tensor_scalar(out=difn[:, l:l + 1], in0=difn[:, l:l + 1],
                                                                 scalar1=1.0 - li, scalar2=None, op0=ALU.mult),
              [r_const], [r_const])

    banks = [es.enter_context(nc.psum_tensor("bank%d" % i, [128, 512], F32)) for i in range(8)]
    rbank = [R() for _ in range(8)]

    NSTG, NWS = 2, 4
    stg = [sb("stg%d" % i, [128, 1024], F32) for i in range(NSTG)]
    r_stg = [R() for _ in range(NSTG)]
    d_stg = [tk.dsem() for _ in range(NSTG)]
    wsl = [sb("wsl%d" % i, [128, 1024], BF16) for i in range(NWS)]
    r_wsl = [R() for _ in range(NWS)]
    ctr = {"stg": 0, "wsl": 0, "bank": 0, "hb": 0, "ev": 0}

    def stage(src_ap, view3=None):
        i = ctr["stg"] % NSTG
        ctr["stg"] += 1
        dst = stg[i][:] if view3 is None else stg[i][:].rearrange("p (a b) -> p a b", a=view3[0], b=view3[1])
        tk.dma(dst, src_ap, [], [r_stg[i]], d_stg[i])
        return stg[i], r_stg[i]

    def cast(eng, out_ap, in_ap, r, w):
        if eng == "act":
            tk.op("act", lambda a: a.activation(out=out_ap, in_=in_ap, func=AF.Copy), r, w)
        else:
            tk.op(eng, lambda g: g.tensor_copy(out=out_ap, in_=in_ap), r, w)

    NSLAB = 68
    wscr = nc.dram_tensor("wscr", [depth * NSLAB, 128, 1024], BF16).ap()
    woscr = nc.dram_tensor("woscr", [depth, 128, 8 * D], BF16).ap()
    wdscr = nc.dram_tensor("wdscr", [depth, 128, NJ * D], BF16).ap()
    d_wsl = [tk.dsem() for _ in range(NWS)]
    d_wst = [tk.dsem() for _ in range(NWS)]
    d_big = tk.dsem()
    cur = {"seq": 0}

    def load_slab(src_ap, eng="pool", key=None):
        j = ctr["wsl"] % NWS
        ctr["wsl"] += 1
        if cur["seq"] == 0:
            st, rst = stage(src_ap, (8, 128))
            cast(eng, wsl[j][:], st[:], [rst], [r_wsl[j]])
            tk.dma(wscr[key], wsl[j][:], [r_wsl[j]], [], d_wst[j])
        else:
            tk.dma(wsl[j][:], wscr[key], [], [r_wsl[j]], d_wsl[j])
        return wsl[j][:].rearrange("p (a b) -> p a b", a=8, b=128), r_wsl[j]

    def wcols(w_ap_l, c0):
        return w_ap_l.rearrange("(kc p) n -> p kc n", p=128)[:, :, c0:c0 + 128]

    def next_bank(lo=0, hi=8):
        i = lo + ctr["bank"] % (hi - lo)
        ctr["bank"] += 1
        return i

    NHB = 3
    hb = [sb("hb%d" % i, [128, D], F32) for i in range(NHB)]
    r_hb = [R() for _ in range(NHB)]
    d_hb = [tk.dsem() for _ in range(NHB)]
    d_hst = [tk.dsem() for _ in range(NHB)]
    NUN = 3
    un = [sb("un%d" % i, [128, D], F32) for i in range(NUN)]
    r_un = [R() for _ in range(NUN)]
    junk = sb("junk", [128, D], BF16)
    r_junk = R()
    gbc = sb("gbc", [128, D], F32)
    r_gbc = R()
    d_gbc = tk.dsem()
    stat = sb("stat", [128, 3 * NTB], F32)
    r_stat = [R() for _ in range(NTB)]

    def blk(ap_seq, tb):
        return ap_seq.rearrange("(tb p) d -> p tb d", p=128)[:, tb, :]

    out_tokens = []

    def norm_stats(i, tb):
        tk.op("act", lambda a: a.activation(out=junk[:], in_=hb[i][:], func=AF.Square,
                                            accum_out=stat[:, tb:tb + 1]),
              [r_hb[i]], [r_junk, r_stat[tb]])
        tk.op("act", lambda a: a.activation(out=stat[:, NTB + tb:NTB + tb + 1], in_=stat[:, tb:tb + 1],
                                            func=AF.Sqrt, scale=1.0 / D, bias=EPS),
              [r_stat[tb]], [r_stat[tb]])
        tk.op("dve", lambda v: v.reciprocal(out=stat[:, 2 * NTB + tb:2 * NTB + tb + 1],
                                            in_=stat[:, NTB + tb:NTB + tb + 1]),
              [r_stat[tb]], [r_stat[tb]])

    uT = sb("uT", [128, 8, S], BF16)
    r_uT = [R() for _ in range(NTB)]

    for seq in range(nseq):
        cur["seq"] = seq
        for l in range(depth):
            skey = {"i": l * NSLAB}

            def nkey():
                skey["i"] += 1
                return skey["i"] - 1
            tk.barrier()
            tk.epoch()
            hsrc = x[seq] if l == 0 else y[seq]
            with ExitStack() as sl:

                def norm_phase(src, g_dram):
                    tk.dma(gbc[:], g_dram, [], [r_gbc], d_gbc)

                    def stage_a(tb):
                        i = ctr["hb"] % NHB
                        ctr["hb"] += 1
                        tk.dma(hb[i][:], blk(src, tb), [], [r_hb[i]], d_hb[i])
                        fused_a(i, tb)

                    stage_a(0)
                    stage_a(1)
                    for tb in range(NTB):
                        if tb + 2 < NTB:
                            stage_a(tb + 2)
                        stage_b(tb)

                if True:
                    def fused_a(i, tb):
                        norm_stats(i, tb)
                        u = tb % NUN
                        tk.op("dve", lambda v: v.scalar_tensor_tensor(
                            out=un[u][:], in0=hb[i][:], scalar=stat[:, 2 * NTB + tb:2 * NTB + tb + 1],
                            in1=gbc[:], op0=ALU.mult, op1=ALU.mult),
                            [r_hb[i], r_stat[tb], r_gbc], [r_un[u]])

                    def stage_b(tb):
                        u = tb % NUN
                        for half in range(2):
                            b = next_bank()

                            def tr(t, b=b, half=half):
                                ins = None
                                for q in range(4):
                                    kc = half * 4 + q
                                    ins = t.transpose(out=banks[b][:, q * 128:(q + 1) * 128],
                                                      in_=un[u][:, kc * 128:(kc + 1) * 128], identity=ident[:])
                                return ins
                            tk.op("pe", tr, [r_un[u], r_const], [rbank[b]])
                            src_ps = banks[b][:].rearrange("p (a b) -> p a b", a=4, b=128)
                            dst = uT[:, half * 4:half * 4 + 4, tb * 128:(tb + 1) * 128]
                            if half == 0:
                                tk.op("act", lambda a, s=src_ps, d=dst: a.activation(out=d, in_=s, func=AF.Copy),
                                      [rbank[b]], [r_uT[tb]])
                            else:
                                tk.op("dve", lambda v, s=src_ps, d=dst: v.tensor_copy(out=d, in_=s),
                                      [rbank[b]], [r_uT[tb]])

                if l == 0:
                    norm_phase(hsrc, attn_bc[l])

                with ExitStack() as att:
                    def asb(name, shape, dt):
                        return att.enter_context(nc.sbuf_tensor("%s_%d_%d" % (name, seq, l), list(shape), dt))
                    mixT = asb("mixT", [128, 8, S], BF16)
                    r_mix = [R() for _ in range(NTB)]
                    QT = asb("QT", [128, S], BF16)
                    QS = asb("QS", [128, S], BF16)
                    r_qs = [R() for _ in range(NT5)]
                    Bt = asb("Bt", [128, 512], F32)
                    lgcol = asb("lgcol", [128, 1], F32)
                    tabT = [asb("tabT%d" % s_, [128, 16], F32) for s_ in range(2)]
                    r_tab = R()
                    KT = asb("KT", [128, S], BF16)
                    Vt = asb("Vt", [128, NTB, 2, 128], BF16)
                    SG = asb("SG", [128, 2, S], BF16)
                    r_q = [R() for _ in range(NT5)]
                    r_k = [R() for _ in range(NT5)]
                    r_v = [R() for _ in range(NT5)]
                    r_sg = [R() for _ in range(NT5)]
                    Mt = [asb("Mt%d" % s_, [128, 896], F32) for s_ in range(2)]
                    Ft = [asb("Ft%d" % s_, [128, 512], F32) for s_ in range(1)]
                    r_dec = [R(), R()]
                    Eb = [asb("Eb%d" % i, [128, 512], F32) for i in range(4)]
                    r_E = [R() for _ in range(4)]
                    Pb = [asb("Pb%d" % i, [128, 512], BF16) for i in range(6)]
                    r_P = [R() for _ in range(6)]
                    ep = [asb("ep%d" % i, [128, 512], F32) for i in range(6)]
                    r_ep = [R() for _ in range(6)]
                    ectr = {"E": 0, "P": 0}

                    def proj_fm(slab, rsl, t5, evac):
                        b = next_bank(0, 4)

                        def mm(t):
                            ins = None
                            for kc in range(8):
                                ins = t.matmul(banks[b][:, :], slab[:, kc, :], uT[:, kc, t5 * 512:(t5 + 1) * 512],
                                               start=(kc == 0), stop=(kc == 7))
                            return ins
                        tk.op("pe", mm, [rsl] + r_uT[t5 * 4:t5 * 4 + 4], [rbank[b]])
                        evac(b)

                    def evac_copy(dst_ap, rdst):
                        def f(b):
                            k = ctr["ev"]
                            ctr["ev"] += 1
                            if k % 2 == 0:
                                tk.op("act", lambda a: a.activation(out=dst_ap, in_=banks[b][:, :], func=AF.Copy),
                                      [rbank[b]], [rdst])
                            else:
                                tk.op("dve", lambda v: v.tensor_copy(out=dst_ap, in_=banks[b][:, :]),
                                      [rbank[b]], [rdst])
                        return f

                    def proj_v(slab, rsl, hh):
                        for g4 in range(NT5):
                            b = next_bank(0, 4)

                            def mm(t):
                                ins = None
                                for q in range(4):
                                    tb = g4 * 4 + q
                                    for kc in range(8):
                                        ins = t.matmul(banks[b][:, q * 128:(q + 1) * 128],
                                                       uT[:, kc, tb * 128:(tb + 1) * 128], slab[:, kc, :],
                                                       start=(kc == 0), stop=(kc == 7))
                                return ins
                            tk.op("pe", mm, [rsl] + r_uT[g4 * 4:g4 * 4 + 4], [rbank[b]])
                            src_ps = banks[b][:].rearrange("p (a b) -> p a b", a=4, b=128)
                            dst = Vt[:, g4 * 4:g4 * 4 + 4, hh, :]
                            k = ctr["ev"]
                            ctr["ev"] += 1
                            if k % 2 == 0:
                                tk.op("act", lambda a, s=src_ps, d=dst: a.activation(out=d, in_=s, func=AF.Copy),
                                      [rbank[b]], [r_v[g4]])
                            else:
                                tk.op("dve", lambda v, s=src_ps, d=dst: v.tensor_copy(out=d, in_=s),
                                      [rbank[b]], [r_v[g4]])

                    units = [("ret", 0), ("ret", 1)] + [("diff", h_) for h_ in range(4)]
                    for kind, ui in units:
                        is_ret = kind == "ret"
                        new_path = is_ret or ui > 0
                        RET_NEW = os.environ.get('KDBG_RET_NEW', '1') == '1'
                        DIFF_NEW = os.environ.get('KDBG_DIFF_NEW', '1') == '1'
                        for s_ in range(2 if is_ret else 1):
                            if is_ret:
                                lg = math.log(ret_gamma(2 * ui + s_))
                                bias = math.log(0.125)
                            else:
                                lg = -alibi_slope(ui)
                                bias = 0.0
                            if is_ret or ui == 0 or not DIFF_NEW:
                                tk.op("act", lambda a, s_=s_, lg=lg, bias=bias: a.activation(
                                    out=Mt[s_][:], in_=dmc[:], func=AF.Exp, scale=lg, bias=bias),
                                    [r_const], [r_dec[s_]])
                                tk.op("pool", lambda g, s_=s_: g.tensor_tensor(out=Mt[s_][:], in0=Mt[s_][:],
                                                                               in1=maskm[:], op=ALU.mult),
                                      [r_const, r_dec[s_]], [r_dec[s_]])
                            if (not is_ret) and (ui == 0 or not DIFF_NEW):
                                tk.op("act", lambda a, s_=s_, lg=lg, bias=bias: a.activation(
                                    out=Ft[s_][:], in_=dfull[:], func=AF.Exp, scale=lg, bias=bias),
                                    [r_const], [r_dec[s_]])
                            if is_ret:
                                tk.op("act", lambda a, s_=s_, lg=lg, bias=bias: a.activation(
                                    out=tabT[s_][:], in_=cdt[:], func=AF.Exp, scale=-lg, bias=bias),
                                    [r_const], [r_tab])
                                tk.op("dve", lambda v, s_=s_, lg=lg: v.memset(lgcol[64 * s_:64 * s_ + 64, :], lg),
                                      [], [r_tab])
                        if is_ret:
                            tk.op("act", lambda a: a.activation(out=Bt[:], in_=cqi[:], func=AF.Exp,
                                                                scale=lgcol[:, 0:1]),
                                  [r_const, r_tab], [r_tab])
                        elif ui > 0:
                            tk.op("dve", lambda v: v.tensor_scalar(out=tabT[0][:], in0=cdt[:],
                                                                   scalar1=float(alibi_slope(ui)), scalar2=None,
                                                                   op0=ALU.mult),
                                  [r_const], [r_tab])
                        if is_ret:
                            qc, kc_, vc, gc = OFF_RQ + 128 * ui, OFF_RK + 128 * ui, OFF_RV + 256 * ui, OFF_RG + 256 * ui
                        else:
                            qc, kc_, vc = OFF_DQ + 128 * ui, OFF_DK + 128 * ui, OFF_DV + 128 * ui
                        slab, rsl = load_slab(wcols(w_in[l], qc), key=nkey())
                        for t5 in range(NT5):
                            if is_ret:
                                def evq(b, t5=t5):
                                    tk.op("act", lambda a: a.activation(out=QT[:, t5 * 512:(t5 + 1) * 512],
                                                                        in_=banks[b][:, :], func=AF.Copy),
                                          [rbank[b]], [r_q[t5]])
                                    tk.op("dve", lambda v: v.tensor_tensor(out=QS[:, t5 * 512:(t5 + 1) * 512],
                                                                           in0=banks[b][:, :], in1=Bt[:], op=ALU.mult),
                                          [r_tab], [rbank[b], r_qs[t5]])
                                proj_fm(slab, rsl, t5, evq)
                            else:
                                proj_fm(slab, rsl, t5, evac_copy(QT[:, t5 * 512:(t5 + 1) * 512], r_q[t5]))
                        slab, rsl = load_slab(wcols(w_in[l], kc_), key=nkey())
                        for t5 in range(NT5):
                            proj_fm(slab, rsl, t5, evac_copy(KT[:, t5 * 512:(t5 + 1) * 512], r_k[t5]))
                        for hh in range(2 if is_ret else 1):
                            slab, rsl = load_slab(wcols(w_in[l], vc + 128 * hh), key=nkey())
                            proj_v(slab, rsl, hh)
                        if is_ret:
                            for hh in range(2):
                                slab, rsl = load_slab(wcols(w_in[l], gc + 128 * hh), key=nkey())
                                for t5 in range(NT5):
                                    def ev(b, hh=hh, t5=t5):
                                        tk.op("act", lambda a: a.activation(
                                            out=SG[:, hh, t5 * 512:(t5 + 1) * 512], in_=banks[b][:, :], func=AF.Silu),
                                            [rbank[b]], [r_sg[t5]])
                                    proj_fm(slab, rsl, t5, ev)

                        ZB = [6, 7]
                        for t5 in range(NT5):
                            OB = [6, 7] if (is_ret and t5 % 2 == 1) else [4, 5]
                            nkb = 4 * (t5 + 1)
                            kb0 = max(0, 4 * t5 - 4) if ((not is_ret) and ui == 0) else 0
                            ring = {"i": 0}

                            def issue_S(b_):
                                sb_ = [(ring["i"] % 2) * 2, (ring["i"] % 2) * 2 + 1]
                                ring["i"] += 1
                                offd = (512 * t5 - 128 * b_) >= 128
                                Qsrc, rq_ = (QS, r_qs[t5]) if (is_ret and offd and RET_NEW) else (QT, r_q[t5])
                                for s_ in range(2):
                                    bk = sb_[s_]
                                    tk.op("pe", lambda t, bk=bk, s_=s_: t.matmul(
                                        banks[bk][:, :], KT[64 * s_:64 * s_ + 64, b_ * 128:(b_ + 1) * 128],
                                        Qsrc[64 * s_:64 * s_ + 64, t5 * 512:(t5 + 1) * 512], start=True, stop=True),
                                        [r_k[b_ // 4], rq_], [rbank[bk]])
                                return sb_

                            def make_P(b_, sb_):
                                delta = 512 * t5 - 128 * b_
                                offd = delta >= 128
                                m = delta // 128 + 3
                                ps = []
                                for s_ in range(2):
                                    ds_ = s_ if is_ret else 0
                                    pi = ectr["P"] % 6
                                    ectr["P"] += 1
                                    bk = sb_[s_]
                                    if is_ret:
                                        if offd and not RET_NEW:
                                            c = ret_gamma(2 * ui + s_) ** delta
                                            tk.op("dve", lambda v, bk=bk, pi=pi, c=c: v.scalar_tensor_tensor(
                                                out=Pb[pi][:], in0=banks[bk][:, :], scalar=float(c), in1=Ft[0][:, :],
                                                op0=ALU.mult, op1=ALU.mult),
                                                [rbank[bk], r_dec[0]], [r_P[pi]])
                                        elif offd:
                                            tk.op("act", lambda a, bk=bk, pi=pi, s_=s_: a.activation(
                                                out=Pb[pi][:], in_=banks[bk][:, :], func=AF.Copy,
                                                scale=tabT[s_][:, m:m + 1]),
                                                [rbank[bk], r_tab], [r_P[pi]])
                                        else:
                                            tile_ap = Mt[ds_][:, 384 + delta:384 + delta + 512]
                                            tk.op("dve", lambda v, bk=bk, pi=pi, tile_ap=tile_ap: v.tensor_tensor(
                                                out=Pb[pi][:], in0=banks[bk][:, :], in1=tile_ap, op=ALU.mult),
                                                [rbank[bk], r_dec[ds_]], [r_P[pi]])
                                    elif ui == 0 or not DIFF_NEW:
                                        tile_ap = Ft[0][:, :] if offd else Mt[0][:, 384 + delta:384 + delta + 512]
                                        cb = -alibi_slope(ui) * delta if offd else 0.0
                                        ei = ectr["E"] % 4
                                        ectr["E"] += 1
                                        tk.op("act", lambda a, bk=bk, ei=ei, cb=cb: a.activation(
                                            out=Eb[ei][:], in_=banks[bk][:, :], func=AF.Exp, scale=0.125, bias=float(cb)),
                                            [rbank[bk]], [r_E[ei]])
                                        tk.op("dve", lambda v, ei=ei, pi=pi, tile_ap=tile_ap: v.tensor_tensor(
                                            out=Pb[pi][:], in0=Eb[ei][:], in1=tile_ap, op=ALU.mult),
                                            [r_E[ei], r_dec[0]], [r_P[pi]])
                                    else:
                                        if offd:
                                            tk.op("act", lambda a, bk=bk, pi=pi: a.activation(
                                                out=Pb[pi][:], in_=banks[bk][:, :], func=AF.Exp, scale=0.125,
                                                bias=tabT[0][:, m:m + 1]),
                                                [rbank[bk], r_tab], [r_P[pi]])
                                        else:
                                            ei = ectr["E"] % 4
                                            ectr["E"] += 1
                                            tk.op("act", lambda a, bk=bk, ei=ei: a.activation(
                                                out=Eb[ei][:], in_=banks[bk][:, :], func=AF.Exp, scale=0.125,
                                                bias=tabT[0][:, m:m + 1]),
                                                [rbank[bk], r_tab], [r_E[ei]])
                                            mk = maskm[:, 384 + delta:384 + delta + 512]
                                            tk.op("dve", lambda v, ei=ei, pi=pi, mk=mk: v.tensor_tensor(
                                                out=Pb[pi][:], in0=Eb[ei][:], in1=mk, op=ALU.mult),
                                                [r_E[ei], r_const], [r_P[pi]])
                                    ps.append(pi)
                                return ps

                            def issue_PV(b_, ps):
                                first, last = b_ == kb0, b_ == nkb - 1
                                for s_ in range(2):
                                    pi = ps[s_]
                                    vh = s_ if is_ret else 0
                                    if not is_ret:
                                        tk.op("pe", lambda t, s_=s_, pi=pi: t.matmul(
                                            banks[ZB[s_]][:, :], ones_b[:, :], Pb[pi][:, :], start=first, stop=last),
                                            [r_P[pi], r_const], [rbank[ZB[s_]]])
                                    tk.op("pe", lambda t, s_=s_, pi=pi, vh=vh: t.matmul(
                                        banks[OB[s_]][:, :], Vt[:, b_, vh, :], Pb[pi][:, :], start=first, stop=last),
                                        [r_P[pi], r_v[b_ // 4]], [rbank[OB[s_]]])

                            prev = None
                            for b_ in range(kb0, nkb):
                                sb_ = issue_S(b_)
                                if prev is not None:
                                    issue_PV(*prev)
                                ps = make_P(b_, sb_)
                                prev = (b_, ps)
                            issue_PV(*prev)

                            qs = slice(t5 * 512, (t5 + 1) * 512)
                            tbs = r_mix[t5 * 4:t5 * 4 + 4]
                            if is_ret:
                                for s_ in range(2):
                                    head = 2 * ui + s_
                                    ob = OB[s_]
                                    e0, e1, e2 = ep[3 * s_], ep[3 * s_ + 1], ep[3 * s_ + 2]
                                    re0, re1, re2 = r_ep[3 * s_], r_ep[3 * s_ + 1], r_ep[3 * s_ + 2]
                                    tk.op("act", lambda a, ob=ob, e0=e0: a.activation(out=e0[:], in_=banks[ob][:, :],
                                                                                      func=AF.Square),
                                          [rbank[ob]], [re0])
                                    zb = next_bank(0, 4)
                                    tk.op("pe", lambda t, zb=zb, e0=e0: t.matmul(banks[zb][:, :], ones_f[:, :], e0[:],
                                                                                 start=True, stop=True),
                                          [re0, r_const], [rbank[zb]])
                                    tk.op("act", lambda a, zb=zb, e1=e1: a.activation(out=e1[:], in_=banks[zb][:, :],
                                                                                      func=AF.Ln, scale=1.0 / 128, bias=EPS),
                                          [rbank[zb]], [re1])
                                    tk.op("act", lambda a, e1=e1: a.activation(out=e1[:], in_=e1[:], func=AF.Exp, scale=-0.5),
                                          [re1], [re1])
                                    tk.op("dve", lambda v, ob=ob, e1=e1, e2=e2: v.scalar_tensor_tensor(
                                        out=e2[:], in0=banks[ob][:, :], scalar=retn[:, l:l + 1], in1=e1[:],
                                        op0=ALU.mult, op1=ALU.mult),
                                        [rbank[ob], re1, r_const], [re2])
                                    tk.op("pool", lambda g, e2=e2, s_=s_, head=head: g.tensor_tensor(
                                        out=mixT[:, head, qs], in0=e2[:], in1=SG[:, s_, qs], op=ALU.mult),
                                        [re2, r_sg[t5]], tbs)
                            else:
                                head = 4 + ui
                                e0, e1, e2, e3, e4, e5 = ep
                                tk.op("dve", lambda v: v.tensor_copy(out=e2[:], in_=banks[OB[0]][:, :]),
                                      [rbank[OB[0]]], [r_ep[2]])
                                tk.op("dve", lambda v: v.tensor_copy(out=e3[:], in_=banks[OB[1]][:, :]),
                                      [rbank[OB[1]]], [r_ep[3]])
                                tk.op("act", lambda a: a.activation(out=e0[:], in_=banks[ZB[0]][:, :], func=AF.Ln),
                                      [rbank[ZB[0]]], [r_ep[0]])
                                tk.op("act", lambda a: a.activation(out=e1[:], in_=banks[ZB[1]][:, :], func=AF.Ln),
                                      [rbank[ZB[1]]], [r_ep[1]])
                                tk.op("act", lambda a: a.activation(out=e0[:], in_=e0[:], func=AF.Exp, scale=-1.0),
                                      [r_ep[0]], [r_ep[0]])
                                tk.op("act", lambda a: a.activation(out=e1[:], in_=e1[:], func=AF.Exp, scale=-1.0),
                                      [r_ep[1]], [r_ep[1]])
                                tk.op("dve", lambda v: v.tensor_tensor(out=e2[:], in0=e2[:], in1=e0[:], op=ALU.mult),
                                      [r_ep[0]], [r_ep[2]])
                                tk.op("dve", lambda v: v.tensor_tensor(out=e3[:], in0=e3[:], in1=e1[:], op=ALU.mult),
                                      [r_ep[1]], [r_ep[3]])
                                tk.op("dve", lambda g: g.scalar_tensor_tensor(
                                    out=e4[:], in0=e3[:], scalar=neglam[:, l:l + 1], in1=e2[:],
                                    op0=ALU.mult, op1=ALU.add),
                                    [r_ep[2], r_ep[3], r_const], [r_ep[4]])
                                tk.op("act", lambda a: a.activation(out=e5[:], in_=e4[:], func=AF.Square),
                                      [r_ep[4]], [r_ep[5]])
                                tk.op("pe", lambda t: t.matmul(banks[ZB[0]][:, :], ones_f[:, :], e5[:],
                                                               start=True, stop=True),
                                      [r_ep[5], r_const], [rbank[ZB[0]]])
                                tk.op("act", lambda a: a.activation(out=e0[:], in_=banks[ZB[0]][:, :], func=AF.Ln,
                                                                    scale=1.0 / 128, bias=EPS),
                                      [rbank[ZB[0]]], [r_ep[0]])
                                tk.op("act", lambda a: a.activation(out=e0[:], in_=e0[:], func=AF.Exp, scale=-0.5),
                                      [r_ep[0]], [r_ep[0]])
                                tk.op("dve", lambda v: v.scalar_tensor_tensor(
                                    out=mixT[:, head, qs], in0=e4[:], scalar=difn[:, l:l + 1], in1=e0[:],
                                    op0=ALU.mult, op1=ALU.mult),
                                    [r_ep[4], r_ep[0], r_const], tbs)

                    tk.barrier()
                    with ExitStack() as oo:
                        Wo = oo.enter_context(nc.sbuf_tensor("Wo_%d_%d" % (seq, l), [128, 8, D], BF16))
                        r_wo = R()
                        wo_flat = Wo[:].rearrange("p a b -> p (a b)")
                        if seq == 0:
                            for kc in range(8):
                                st, rst = stage(w_out[l][kc * 128:(kc + 1) * 128, :])
                                cast("act" if kc % 2 == 0 else "dve", Wo[:, kc, :], st[:], [rst], [r_wo])
                            tk.dma(woscr[l], wo_flat, [r_wo], [], d_big)
                        else:
                            tk.dma(wo_flat, woscr[l], [], [r_wo], d_big)
                        tk.dma(gbc[:], ffn_bc[l], [], [r_gbc], d_gbc)
                        for tb in range(NTB):
                            i = ctr["hb"] % NHB
                            ctr["hb"] += 1
                            tk.dma(hb[i][:], blk(hsrc, tb), [], [r_hb[i]], d_hb[i])
                            for nh in range(2):
                                b = next_bank()

                                def mm(t, b=b, nh=nh):
                                    ins = None
                                    for kc in range(8):
                                        ins = t.matmul(banks[b][:, :], mixT[:, kc, tb * 128:(tb + 1) * 128],
                                                       Wo[:, kc, nh * 512:(nh + 1) * 512], start=(kc == 0), stop=(kc == 7))
                                    return ins
                                tk.op("pe", mm, [r_wo, r_mix[tb]], [rbank[b]])
                                tk.op("dve", lambda v, b=b, nh=nh: v.tensor_tensor(
                                    out=hb[i][:, nh * 512:(nh + 1) * 512], in0=hb[i][:, nh * 512:(nh + 1) * 512],
                                    in1=banks[b][:, :], op=ALU.add),
                                    [rbank[b], r_hb[i]], [r_hb[i]])
                            tk.dma(blk(y[seq], tb), hb[i][:], [r_hb[i]], [], d_hst[i])
                            fused_a(i, tb)
                            if tb > 1:
                                stage_b(tb - 2)
                        stage_b(NTB - 2)
                        stage_b(NTB - 1)
                        tk.barrier()
                    tk.barrier()

                with ExitStack() as ff:
                    HT = S // 2
                    gT = ff.enter_context(nc.sbuf_tensor("gT_%d_%d" % (seq, l), [128, NJ, HT], BF16))
                    r_g = [R() for _ in range(NTB)]
                    Wd = ff.enter_context(nc.sbuf_tensor("Wd_%d_%d" % (seq, l), [128, NJ, D], BF16))
                    r_wd = R()
                    sgb = [ff.enter_context(nc.sbuf_tensor("sgb%d_%d_%d" % (i, seq, l), [128, 512], F32))
                           for i in range(2)]
                    r_sgb = [R(), R()]
                    k_sg = 0
                    last = (l == depth - 1)
                    if last:
                        tk.dma(gbc[:], final_bc[:, :], [], [r_gbc], d_gbc)
                    else:
                        tk.dma(gbc[:], attn_bc[l + 1], [], [r_gbc], d_gbc)
                    wd_flat = Wd[:].rearrange("p a b -> p (a b)")
                    if seq > 0:
                        tk.dma(wd_flat, wdscr[l], [], [r_wd], d_big)
                    for hf in range(2):
                        if hf == 1 and seq == 0:
                            tk.dma(wdscr[l], wd_flat, [r_wd], [], d_big)
                        for j in range(NJ):
                            slg, rg_ = load_slab(wcols(w_gate[l], j * 128), "act", key=l * NSLAB + 24 + j)
                            slu, ru_ = load_slab(wcols(w_up[l], j * 128), "dve", key=l * NSLAB + 46 + j)
                            if hf == 0 and seq == 0:
                                st, rst = stage(w_down[l][j * 128:(j + 1) * 128, :])
                                cast("pool", Wd[:, j, :], st[:], [rst], [r_wd])
                            for t5 in (2 * hf, 2 * hf + 1):
                                bg = next_bank()
                                bu = next_bank()

                                def mmg(t, bb=bg, slab=slg, t5=t5):
                                    ins = None
                                    for kc in range(8):
                                        ins = t.matmul(banks[bb][:, :], slab[:, kc, :],
                                                       uT[:, kc, t5 * 512:(t5 + 1) * 512],
                                                       start=(kc == 0), stop=(kc == 7))
                                    return ins
                                tk.op("pe", mmg, [rg_] + r_uT[t5 * 4:t5 * 4 + 4], [rbank[bg]])

                                def mmu(t, bb=bu, slab=slu, t5=t5):
                                    ins = None
                                    for kc in range(8):
                                        ins = t.matmul(banks[bb][:, :], slab[:, kc, :],
                                                       uT[:, kc, t5 * 512:(t5 + 1) * 512],
                                                       start=(kc == 0), stop=(kc == 7))
                                    return ins
                                tk.op("pe", mmu, [ru_] + r_uT[t5 * 4:t5 * 4 + 4], [rbank[bu]])
                                si = k_sg % 2
                                k_sg += 1
                                tk.op("act", lambda a, si=si, bg=bg: a.activation(out=sgb[si][:], in_=banks[bg][:, :],
                                                                                  func=AF.Silu),
                                      [rbank[bg]], [r_sgb[si]])
                                lo = (t5 - 2 * hf) * 512
                                tk.op("dve", lambda v, si=si, bu=bu, j=j, lo=lo: v.tensor_tensor(
                                    out=gT[:, j, lo:lo + 512], in0=sgb[si][:], in1=banks[bu][:, :], op=ALU.mult),
                                    [r_sgb[si], rbank[bu]], r_g[t5 * 4:t5 * 4 + 4])
                        for tb in range(8 * hf, 8 * hf + 8):
                            i = ctr["hb"] % NHB
                            ctr["hb"] += 1
                            tk.dma(hb[i][:], blk(y[seq], tb), [], [r_hb[i]], d_hb[i])
                            lo = (tb - 8 * hf) * 128
                            for nh in range(2):
                                b = next_bank()

                                def mm(t, b=b, nh=nh, lo=lo):
                                    ins = None
                                    for j in range(NJ):
                                        ins = t.matmul(banks[b][:, :], gT[:, j, lo:lo + 128],
                                                       Wd[:, j, nh * 512:(nh + 1) * 512],
                                                       start=(j == 0), stop=(j == NJ - 1))
                                    return ins
                                tk.op("pe", mm, [r_wd, r_g[tb]], [rbank[b]])
                                tk.op("dve", lambda v, b=b, nh=nh: v.tensor_tensor(
                                    out=hb[i][:, nh * 512:(nh + 1) * 512], in0=hb[i][:, nh * 512:(nh + 1) * 512],
                                    in1=banks[b][:, :], op=ALU.add),
                                    [rbank[b], r_hb[i]], [r_hb[i]])
                            if last:
                                norm_stats(i, tb)
                                tk.op("dve", lambda v: v.scalar_tensor_tensor(
                                    out=hb[i][:], in0=hb[i][:], scalar=stat[:, 2 * NTB + tb:2 * NTB + tb + 1],
                                    in1=gbc[:], op0=ALU.mult, op1=ALU.mult),
                                    [r_hb[i], r_stat[tb], r_gbc], [r_hb[i]])
                            tok = tk.dma(blk(y[seq], tb), hb[i][:], [r_hb[i]], [], d_hst[i])
                            if last:
                                out_tokens.append(tok)
                            else:
                                fused_a(i, tb)
                                if tb > 8 * hf + 1:
                                    stage_b(tb - 2)
                        if not last:
                            stage_b(8 * hf + 6)
                            stage_b(8 * hf + 7)
                    tk.barrier()
                tk.barrier()
    tk.finish(out_tokens)
    tk.barrier()
    es.close()
    return nc


def host_consts():
    p = np.arange(128, dtype=np.float32)[:, None]
    c = np.arange(896, dtype=np.float32)[None, :]
    dm = (c - 384.0 - p).astype(np.float32)
    df = (np.arange(512, dtype=np.float32)[None, :] - p).astype(np.float32)
    ident = np.eye(128, dtype=np.float32)
    dt_ = (p - 128.0 * (np.arange(16, dtype=np.float32)[None, :] - 3.0)).astype(np.float32)
    qi = np.ascontiguousarray(np.broadcast_to(np.arange(512, dtype=np.float32)[None, :], (128, 512)))
    return ident, dm, df, dt_, qi


_CACHE = {}


def run(inputs, nseq, depth, ncores, trace=False):
    key = (nseq, depth)
    if key not in _CACHE:
        _CACHE[key] = build_program(nseq, depth)
    nc = _CACHE[key]
    f = lambda a: np.ascontiguousarray(np.asarray(a, dtype=np.float32))
    ident, dm, df, dt_, qi_ = host_consts()
    bc = lambda a: np.ascontiguousarray(np.broadcast_to(f(a)[:, None, :], (a.shape[0], 128, a.shape[1])))
    lam = np.concatenate([f(inputs["lambda_q1"])[:, None, :], f(inputs["lambda_k1"])[:, None, :],
                          f(inputs["lambda_q2"])[:, None, :], f(inputs["lambda_k2"])[:, None, :]], axis=1)
    lam = lam[:depth].reshape(1, depth * 4 * 64)
    shared = {
        "w_in": f(inputs["w_in"])[:depth], "w_out": f(inputs["w_out"])[:depth],
        "w_gate": f(inputs["w_gate"])[:depth], "w_up": f(inputs["w_up"])[:depth],
        "w_down": f(inputs["w_down"])[:depth],
        "attn_bc": bc(f(inputs["attn_norm"])[:depth]), "ffn_bc": bc(f(inputs["ffn_norm"])[:depth]),
        "final_bc": np.ascontiguousarray(np.broadcast_to(f(inputs["final_norm"])[None, :], (128, D))),
        "retn_col": np.ascontiguousarray(f(inputs["ret_norm"])[:depth].T),
        "difn_col": np.ascontiguousarray(f(inputs["diff_norm"])[:depth].T),
        "lam_in": np.ascontiguousarray(np.broadcast_to(lam, (128, depth * 4 * 64))),
        "c_ident": ident, "c_dm": dm, "c_df": df, "c_dt": dt_, "c_qi": qi_,
    }
    xs = f(inputs["x"])
    in_maps = []
    for c in range(ncores):
        m = dict(shared)
        m["x"] = np.ascontiguousarray(xs[c * nseq:(c + 1) * nseq])
        in_maps.append(m)
    res = run_bass_kernel_spmd(nc, in_maps, core_ids=list(range(ncores)), **({"trace": True} if trace else {}))
    out = np.concatenate([r["y"] for r in res.results], axis=0)
    return out, res


def kernel(x, attn_norm, w_in, ret_norm, lambda_q1, lambda_k1, lambda_q2, lambda_k2,
           diff_norm, w_out, ffn_norm, w_gate, w_up, w_down, final_norm):
    inputs = dict(x=x, attn_norm=attn_norm, w_in=w_in, ret_norm=ret_norm, lambda_q1=lambda_q1,
                  lambda_k1=lambda_k1, lambda_q2=lambda_q2, lambda_k2=lambda_k2, diff_norm=diff_norm,
                  w_out=w_out, ffn_norm=ffn_norm, w_gate=w_gate, w_up=w_up, w_down=w_down,
                  final_norm=final_norm)
    out, _ = run(inputs, BATCH // NCORES, DEPTH, NCORES)
    return out.astype(np.float32)
```

```python
import math
import os
from contextlib import ExitStack

import numpy as np
import concourse.bass as bass
import concourse.mybir as mybir
from concourse.bass_utils import run_bass_kernel_spmd

F32 = mybir.dt.float32
BF16 = mybir.dt.bfloat16
AF = mybir.ActivationFunctionType
ALU = mybir.AluOpType
AX = mybir.AxisListType

D = 1024
S = 2048
DEPTH = 2
BATCH = 32
NCORES = 8
FF = 2816
NJ = FF // 128
INW = 3072
EPS = 1e-6
OFF_RQ, OFF_RK, OFF_RV, OFF_RG, OFF_DQ, OFF_DK, OFF_DV = 0, 256, 512, 1024, 1536, 2048, 2560
NTB = S // 128
NT5 = S // 512


class R:
    __slots__ = ("w", "rs")

    def __init__(self):
        self.w = None
        self.rs = {}


class Eng:
    def __init__(self, name, handle):
        self.name = name
        self.h = handle
        self.sem = None
        self.count = 0
        self.known = {}


class DSem:
    def __init__(self, sem):
        self.sem = sem
        self.count = 0


class Tracker:
    def __init__(self, nc, es):
        self.nc = nc
        self.es = es
        self.nsem = 0
        self.engs = {
            "pe": Eng("pe", nc.tensor),
            "act": Eng("act", nc.scalar),
            "dve": Eng("dve", nc.vector),
            "pool": Eng("pool", nc.gpsimd),
            "sp": Eng("sp", nc.sync),
        }
        self.dsems = []
        self.epoch()

    def new_sem(self):
        self.nsem += 1
        return self.es.enter_context(self.nc.semaphore("ts%d" % self.nsem))

    def epoch(self):
        for e in self.engs.values():
            if e.name == "sp":
                continue
            e.sem = self.new_sem()
            e.count = 0

    def dsem(self):
        d = DSem(self.new_sem())
        self.dsems.append(d)
        return d

    def _waits(self, e, r, w):
        waits = {}

        def need(tok):
            if tok is None:
                return
            sem, val = tok
            if e.name == "pe" and sem is e.sem:
                return
            if e.known.get(sem, 0) >= val:
                return
            if waits.get(sem, (None, 0))[1] < val:
                waits[sem] = (sem, val)

        for x in r:
            need(x.w)
        for x in w:
            need(x.w)
            for t in x.rs.values():
                need(t)
        for sem, val in waits.values():
            e.known[sem] = val
            e.h.wait_ge(sem, val)

    @staticmethod
    def _mark(tok, r, w):
        for x in r:
            old = x.rs.get(tok[0])
            if old is None or old[1] < tok[1]:
                x.rs[tok[0]] = tok
        for x in w:
            x.w = tok
            x.rs = {}

    def op(self, eng, fn, r=(), w=()):
        e = self.engs[eng]
        self._waits(e, r, w)
        ins = fn(e.h)
        e.count += 1
        tok = (e.sem, e.count)
        ins.then_inc(e.sem, 1)
        self._mark(tok, r, w)
        return tok

    def dma(self, out, in_, r, w, ds):
        e = self.engs["sp"]
        self._waits(e, r, w)
        ins = e.h.dma_start(out=out, in_=in_)
        ds.count += 16
        tok = (ds.sem, ds.count)
        ins.then_inc(ds.sem, 16)
        self._mark(tok, r, w)
        return tok

    def barrier(self):
        toks = []
        for e in self.engs.values():
            if e.name != "sp" and e.count > 0:
                toks.append((e.sem, e.count))
        for d in self.dsems:
            if d.count > 0:
                toks.append((d.sem, d.count))
        for e in self.engs.values():
            for sem, val in toks:
                if sem is e.sem:
                    continue
                if e.known.get(sem, 0) >= val:
                    continue
                e.known[sem] = val
                e.h.wait_ge(sem, val)

    def finish(self, toks):
        e = self.engs["sp"]
        for sem, val in toks:
            if e.known.get(sem, 0) >= val:
                continue
            e.known[sem] = val
            e.h.wait_ge(sem, val)


def ret_gamma(h):
    return 1.0 - 2.0 ** (-5.0 - h)


def alibi_slope(h):
    return 2.0 ** (-8.0 * (h + 1) / 4.0)


def build_program(nseq, depth):
    nc = bass.Bass("TRN2", target_bir_lowering=False)
    es = ExitStack()

    def dram(name, shape, kind="ExternalInput", dt=F32):
        return nc.dram_tensor(name, list(shape), dt, kind=kind).ap()

    x = dram("x", [nseq, S, D])
    y = dram("y", [nseq, S, D], kind="ExternalOutput")
    w_in = dram("w_in", [depth, D, INW])
    w_out = dram("w_out", [depth, D, D])
    w_gate = dram("w_gate", [depth, D, FF])
    w_up = dram("w_up", [depth, D, FF])
    w_down = dram("w_down", [depth, FF, D])
    attn_bc = dram("attn_bc", [depth, 128, D])
    ffn_bc = dram("ffn_bc", [depth, 128, D])
    final_bc = dram("final_bc", [128, D])
    retn_col = dram("retn_col", [128, depth])
    difn_col = dram("difn_col", [128, depth])
    lam_in = dram("lam_in", [128, depth * 4 * 64])
    c_ident = dram("c_ident", [128, 128])
    c_dm = dram("c_dm", [128, 896])
    c_df = dram("c_df", [128, 512])
    c_dt = dram("c_dt", [128, 16])
    c_qi = dram("c_qi", [128, 512])

    tk = Tracker(nc, es)

    def sb(name, shape, dt):
        return es.enter_context(nc.sbuf_tensor(name, list(shape), dt))

    ident = sb("ident", [128, 128], F32)
    ones_f = sb("ones_f", [128, 128], F32)
    ones_b = sb("ones_b", [128, 128], BF16)
    dmc = sb("dmc", [128, 896], F32)
    maskm = sb("maskm", [128, 896], F32)
    dfull = sb("dfull", [128, 512], F32)
    cdt = sb("cdt", [128, 16], F32)
    cqi = sb("cqi", [128, 512], F32)
    retn = sb("retn", [128, depth], F32)
    difn = sb("difn", [128, depth], F32)
    lamt = sb("lamt", [128, depth * 4 * 64], F32)
    lamw = sb("lamw", [128, 2 * 64], F32)
    lams = sb("lams", [128, 8], F32)
    neglam = sb("neglam", [128, depth], F32)
    r_const = R()
    r_lam = R()
    cds = tk.dsem()
    tk.dma(ident[:], c_ident[:, :], [], [r_const], cds)
    tk.dma(dmc[:], c_dm[:, :], [], [r_const], cds)
    tk.dma(dfull[:], c_df[:, :], [], [r_const], cds)
    tk.dma(cdt[:], c_dt[:, :], [], [r_const], cds)
    tk.dma(cqi[:], c_qi[:, :], [], [r_const], cds)
    tk.dma(retn[:], retn_col[:, :], [], [r_const], cds)
    tk.dma(difn[:], difn_col[:, :], [], [r_const], cds)
    tk.dma(lamt[:], lam_in[:, :], [], [r_const], cds)
    r_const.w = (cds.sem, cds.count)
    tk.op("dve", lambda v: v.memset(ones_f[:], 1.0), [], [r_const])
    tk.op("dve", lambda v: v.memset(ones_b[:], 1.0), [], [r_const])
    tk.op("dve", lambda v: v.tensor_single_scalar(out=maskm[:], in_=dmc[:], scalar=0.0, op=ALU.is_ge),
          [r_const], [r_const])
    tk.op("dve", lambda v: v.tensor_scalar_max(out=dmc[:], in0=dmc[:], scalar1=0.0), [r_const], [r_const])
    for l in range(depth):
        base = l * 256
        tk.op("dve", lambda v, b=base: v.tensor_tensor(out=lamw[:, 0:64], in0=lamt[:, b:b + 64],
                                                       in1=lamt[:, b + 64:b + 128], op=ALU.mult),
              [r_const], [r_lam])
        tk.op("dve", lambda v, b=base: v.tensor_tensor(out=lamw[:, 64:128], in0=lamt[:, b + 128:b + 192],
                                                       in1=lamt[:, b + 192:b + 256], op=ALU.mult),
              [r_const], [r_lam])
        tk.op("dve", lambda v: v.reduce_sum(out=lams[:, 0:1], in_=lamw[:, 0:64], axis=AX.X), [r_lam], [r_lam])
        tk.op("dve", lambda v: v.reduce_sum(out=lams[:, 1:2], in_=lamw[:, 64:128], axis=AX.X), [r_lam], [r_lam])
        tk.op("act", lambda a: a.activation(out=lams[:, 2:4], in_=lams[:, 0:2], func=AF.Exp), [r_lam], [r_lam])
        lam_init = 0.8 - 0.6 * math.exp(-0.3 * l)
        tk.op("dve", lambda v, li=lam_init: v.tensor_scalar(out=lams[:, 4:5], in0=lams[:, 3:4], scalar1=-li,
                                                            scalar2=None, op0=ALU.add), [r_lam], [r_lam])
        tk.op("dve", lambda v, l=l: v.tensor_tensor(out=neglam[:, l:l + 1], in0=lams[:, 4:5], in1=lams[:, 2:3],
                                                    op=ALU.subtract), [r_lam], [r_const])
        tk.op("dve", lambda v, l=l, li=lam_init: v.tensor_scalar(out=difn[:, l:l + 1], in0=difn[:, l:l + 1],
                                                                 scalar1=1.0 - li, scalar2=None, op0=ALU.mult),
              [r_const], [r_const])

    banks = [es.enter_context(nc.psum_tensor("bank%d" % i, [128, 512], F32)) for i in range(8)]
    rbank = [R() for _ in range(8)]

    NSTG, NWS = 2, 4
    stg = [sb("stg%d" % i, [128, 1024], F32) for i in range(NSTG)]
    r_stg = [R() for _ in range(NSTG)]
    d_stg = [tk.dsem() for _ in range(NSTG)]
    wsl = [sb("wsl%d" % i, [128, 1024], BF16) for i in range(NWS)]
    r_wsl = [R() for _ in range(NWS)]
    ctr = {"stg": 0, "wsl": 0, "bank": 0, "hb": 0, "ev": 0}

    def stage(src_ap, view3=None):
        i = ctr["stg"] % NSTG
        ctr["stg"] += 1
        dst = stg[i][:] if view3 is None else stg[i][:].rearrange("p (a b) -> p a b", a=view3[0], b=view3[1])
        tk.dma(dst, src_ap, [], [r_stg[i]], d_stg[i])
        return stg[i], r_stg[i]

    def cast(eng, out_ap, in_ap, r, w):
        if eng == "act":
            tk.op("act", lambda a: a.activation(out=out_ap, in_=in_ap, func=AF.Copy), r, w)
        else:
            tk.op(eng, lambda g: g.tensor_copy(out=out_ap, in_=in_ap), r, w)

    NSLAB = 68
    wscr = nc.dram_tensor("wscr", [depth * NSLAB, 128, 1024], BF16).ap()
    woscr = nc.dram_tensor("woscr", [depth, 128, 8 * D], BF16).ap()
    wdscr = nc.dram_tensor("wdscr", [depth, 128, NJ * D], BF16).ap()
    d_wsl = [tk.dsem() for _ in range(NWS)]
    d_wst = [tk.dsem() for _ in range(NWS)]
    d_big = tk.dsem()
    cur = {"seq": 0}

    def load_slab(src_ap, eng="pool", key=None):
        j = ctr["wsl"] % NWS
        ctr["wsl"] += 1
        if cur["seq"] == 0:
            st, rst = stage(src_ap, (8, 128))
            cast(eng, wsl[j][:], st[:], [rst], [r_wsl[j]])
            tk.dma(wscr[key], wsl[j][:], [r_wsl[j]], [], d_wst[j])
        else:
            tk.dma(wsl[j][:], wscr[key], [], [r_wsl[j]], d_wsl[j])
        return wsl[j][:].rearrange("p (a b) -> p a b", a=8, b=128), r_wsl[j]

    def wcols(w_ap_l, c0):
        return w_ap_l.rearrange("(kc p) n -> p kc n", p=128)[:, :, c0:c0 + 128]

    def next_bank(lo=0, hi=8):
        i = lo + ctr["bank"] % (hi - lo)
        ctr["bank"] += 1
        return i

    NHB = 3
    hb = [sb("hb%d" % i, [128, D], F32) for i in range(NHB)]
    r_hb = [R() for _ in range(NHB)]
    d_hb = [tk.dsem() for _ in range(NHB)]
    d_hst = [tk.dsem() for _ in range(NHB)]
    NUN = 3
    un = [sb("un%d" % i, [128, D], F32) for i in range(NUN)]
    r_un = [R() for _ in range(NUN)]
    junk = sb("junk", [128, D], BF16)
    r_junk = R()
    gbc = sb("gbc", [128, D], F32)
    r_gbc = R()
    d_gbc = tk.dsem()
    stat = sb("stat", [128, 3 * NTB], F32)
    r_stat = [R() for _ in range(NTB)]

    def blk(ap_seq, tb):
        return ap_seq.rearrange("(tb p) d -> p tb d", p=128)[:, tb, :]

    out_tokens = []

    def norm_stats(i, tb):
        tk.op("act", lambda a: a.activation(out=junk[:], in_=hb[i][:], func=AF.Square,
                                            accum_out=stat[:, tb:tb + 1]),
              [r_hb[i]], [r_junk, r_stat[tb]])
        tk.op("act", lambda a: a.activation(out=stat[:, NTB + tb:NTB + tb + 1], in_=stat[:, tb:tb + 1],
                                            func=AF.Sqrt, scale=1.0 / D, bias=EPS),
              [r_stat[tb]], [r_stat[tb]])
        tk.op("dve", lambda v: v.reciprocal(out=stat[:, 2 * NTB + tb:2 * NTB + tb + 1],
                                            in_=stat[:, NTB + tb:NTB + tb + 1]),
              [r_stat[tb]], [r_stat[tb]])

    uT = sb("uT", [128, 8, S], BF16)
    r_uT = [R() for _ in range(NTB)]

    for seq in range(nseq):
        cur["seq"] = seq
        for l in range(depth):
            skey = {"i": l * NSLAB}

            def nkey():
                skey["i"] += 1
                return skey["i"] - 1
            tk.barrier()
            tk.epoch()
            hsrc = x[seq] if l == 0 else y[seq]
            with ExitStack() as sl:

                def norm_phase(src, g_dram):
                    tk.dma(gbc[:], g_dram, [], [r_gbc], d_gbc)

                    def stage_a(tb):
                        i = ctr["hb"] % NHB
                        ctr["hb"] += 1
                        tk.dma(hb[i][:], blk(src, tb), [], [r_hb[i]], d_hb[i])
                        fused_a(i, tb)

                    stage_a(0)
                    stage_a(1)
                    for tb in range(NTB):
                        if tb + 2 < NTB:
                            stage_a(tb + 2)
                        stage_b(tb)

                if True:
                    def fused_a(i, tb):
                        norm_stats(i, tb)
                        u = tb % NUN
                        tk.op("dve", lambda v: v.scalar_tensor_tensor(
                            out=un[u][:], in0=hb[i][:], scalar=stat[:, 2 * NTB + tb:2 * NTB + tb + 1],
                            in1=gbc[:], op0=ALU.mult, op1=ALU.mult),
                            [r_hb[i], r_stat[tb], r_gbc], [r_un[u]])

                    def stage_b(tb):
                        u = tb % NUN
                        for half in range(2):
                            b = next_bank()

                            def tr(t, b=b, half=half):
                                ins = None
                                for q in range(4):
                                    kc = half * 4 + q
                                    ins = t.transpose(out=banks[b][:, q * 128:(q + 1) * 128],
                                                      in_=un[u][:, kc * 128:(kc + 1) * 128], identity=ident[:])
                                return ins
                            tk.op("pe", tr, [r_un[u], r_const], [rbank[b]])
                            src_ps = banks[b][:].rearrange("p (a b) -> p a b", a=4, b=128)
                            dst = uT[:, half * 4:half * 4 + 4, tb * 128:(tb + 1) * 128]
                            if half == 0:
                                tk.op("act", lambda a, s=src_ps, d=dst: a.activation(out=d, in_=s, func=AF.Copy),
                                      [rbank[b]], [r_uT[tb]])
                            else:
                                tk.op("dve", lambda v, s=src_ps, d=dst: v.tensor_copy(out=d, in_=s),
                                      [rbank[b]], [r_uT[tb]])

                if l == 0:
                    norm_phase(hsrc, attn_bc[l])

                with ExitStack() as att:
                    def asb(name, shape, dt):
                        return att.enter_context(nc.sbuf_tensor("%s_%d_%d" % (name, seq, l), list(shape), dt))
                    mixT = asb("mixT", [128, 8, S], BF16)
                    r_mix = [R() for _ in range(NTB)]
                    QT = asb("QT", [128, S], BF16)
                    QS = asb("QS", [128, S], BF16)
                    r_qs = [R() for _ in range(NT5)]
                    Bt = asb("Bt", [128, 512], F32)
                    lgcol = asb("lgcol", [128, 1], F32)
                    tabT = [asb("tabT%d" % s_, [128, 16], F32) for s_ in range(2)]
                    r_tab = R()
                    KT = asb("KT", [128, S], BF16)
                    Vt = asb("Vt", [128, NTB, 2, 128], BF16)
                    SG = asb("SG", [128, 2, S], BF16)
                    r_q = [R() for _ in range(NT5)]
                    r_k = [R() for _ in range(NT5)]
                    r_v = [R() for _ in range(NT5)]
                    r_sg = [R() for _ in range(NT5)]
                    Mt = [asb("Mt%d" % s_, [128, 896], F32) for s_ in range(2)]
                    Ft = [asb("Ft%d" % s_, [128, 512], F32) for s_ in range(1)]
                    r_dec = [R(), R()]
                    Eb = [asb("Eb%d" % i, [128, 512], F32) for i in range(4)]
                    r_E = [R() for _ in range(4)]
                    Pb = [asb("Pb%d" % i, [128, 512], BF16) for i in range(6)]
                    r_P = [R() for _ in range(6)]
                    ep = [asb("ep%d" % i, [128, 512], F32) for i in range(6)]
                    r_ep = [R() for _ in range(6)]
                    ectr = {"E": 0, "P": 0}
                    pending = []

                    def flush():
                        while pending:
                            pending.pop(0)()

                    def ret_part2(s_, head, ob, qs, tbs, rsg, l_):
                        e0, e1, e2 = ep[3 * s_], ep[3 * s_ + 1], ep[3 * s_ + 2]
                        re0, re1, re2 = r_ep[3 * s_], r_ep[3 * s_ + 1], r_ep[3 * s_ + 2]

                        def run():
                            zb = next_bank(0, 4)
                            tk.op("pe", lambda t: t.matmul(banks[zb][:, :], ones_f[:, :], e0[:], start=True, stop=True),
                                  [re0, r_const], [rbank[zb]])
                            tk.op("act", lambda a: a.activation(out=e1[:], in_=banks[zb][:, :], func=AF.Ln,
                                                                scale=1.0 / 128, bias=EPS),
                                  [rbank[zb]], [re1])
                            tk.op("act", lambda a: a.activation(out=e1[:], in_=e1[:], func=AF.Exp, scale=-0.5),
                                  [re1], [re1])
                            tk.op("dve", lambda v: v.scalar_tensor_tensor(
                                out=e2[:], in0=banks[ob][:, :], scalar=retn[:, l_:l_ + 1], in1=e1[:],
                                op0=ALU.mult, op1=ALU.mult),
                                [rbank[ob], re1, r_const], [re2])
                            tk.op("pool", lambda g: g.tensor_tensor(
                                out=mixT[:, head, qs], in0=e2[:], in1=SG[:, s_, qs], op=ALU.mult),
                                [re2, rsg], tbs)
                        return run

                    def diff_part2(head, qs, tbs, l_):
                        e0, e4, e5 = ep[0], ep[4], ep[5]

                        def run():
                            zb = next_bank(0, 4)
                            tk.op("pe", lambda t: t.matmul(banks[zb][:, :], ones_f[:, :], e5[:], start=True, stop=True),
                                  [r_ep[5], r_const], [rbank[zb]])
                            tk.op("act", lambda a: a.activation(out=e0[:], in_=banks[zb][:, :], func=AF.Ln,
                                                                scale=1.0 / 128, bias=EPS),
                                  [rbank[zb]], [r_ep[0]])
                            tk.op("act", lambda a: a.activation(out=e0[:], in_=e0[:], func=AF.Exp, scale=-0.5),
                                  [r_ep[0]], [r_ep[0]])
                            tk.op("dve", lambda v: v.scalar_tensor_tensor(
                                out=mixT[:, head, qs], in0=e4[:], scalar=difn[:, l_:l_ + 1], in1=e0[:],
                                op0=ALU.mult, op1=ALU.mult),
                                [r_ep[4], r_ep[0], r_const], tbs)
                        return run

                    def proj_fm(slab, rsl, t5, evac):
                        b = next_bank(0, 4)

                        def mm(t):
                            ins = None
                            for kc in range(8):
                                ins = t.matmul(banks[b][:, :], slab[:, kc, :], uT[:, kc, t5 * 512:(t5 + 1) * 512],
                                               start=(kc == 0), stop=(kc == 7))
                            return ins
                        tk.op("pe", mm, [rsl] + r_uT[t5 * 4:t5 * 4 + 4], [rbank[b]])
                        evac(b)

                    def evac_copy(dst_ap, rdst):
                        def f(b):
                            k = ctr["ev"]
                            ctr["ev"] += 1
                            if k % 2 == 0:
                                tk.op("act", lambda a: a.activation(out=dst_ap, in_=banks[b][:, :], func=AF.Copy),
                                      [rbank[b]], [rdst])
                            else:
                                tk.op("dve", lambda v: v.tensor_copy(out=dst_ap, in_=banks[b][:, :]),
                                      [rbank[b]], [rdst])
                        return f

                    def proj_v(slab, rsl, hh):
                        for g4 in range(NT5):
                            b = next_bank(0, 4)

                            def mm(t):
                                ins = None
                                for q in range(4):
                                    tb = g4 * 4 + q
                                    for kc in range(8):
                                        ins = t.matmul(banks[b][:, q * 128:(q + 1) * 128],
                                                       uT[:, kc, tb * 128:(tb + 1) * 128], slab[:, kc, :],
                                                       start=(kc == 0), stop=(kc == 7))
                                return ins
                            tk.op("pe", mm, [rsl] + r_uT[g4 * 4:g4 * 4 + 4], [rbank[b]])
                            src_ps = banks[b][:].rearrange("p (a b) -> p a b", a=4, b=128)
                            dst = Vt[:, g4 * 4:g4 * 4 + 4, hh, :]
                            k = ctr["ev"]
                            ctr["ev"] += 1
                            if k % 2 == 0:
                                tk.op("act", lambda a, s=src_ps, d=dst: a.activation(out=d, in_=s, func=AF.Copy),
                                      [rbank[b]], [r_v[g4]])
                            else:
                                tk.op("dve", lambda v, s=src_ps, d=dst: v.tensor_copy(out=d, in_=s),
                                      [rbank[b]], [r_v[g4]])

                    units = [("ret", 0), ("ret", 1)] + [("diff", h_) for h_ in range(4)]
                    for kind, ui in units:
                        is_ret = kind == "ret"
                        new_path = is_ret or ui > 0
                        RET_NEW = os.environ.get('KDBG_RET_NEW', '1') == '1'
                        DIFF_NEW = os.environ.get('KDBG_DIFF_NEW', '1') == '1'
                        for s_ in range(2 if is_ret else 1):
                            if is_ret:
                                lg = math.log(ret_gamma(2 * ui + s_))
                                bias = math.log(0.125)
                            else:
                                lg = -alibi_slope(ui)
                                bias = 0.0
                            if is_ret or ui == 0 or not DIFF_NEW:
                                tk.op("act", lambda a, s_=s_, lg=lg, bias=bias: a.activation(
                                    out=Mt[s_][:], in_=dmc[:], func=AF.Exp, scale=lg, bias=bias),
                                    [r_const], [r_dec[s_]])
                                tk.op("pool", lambda g, s_=s_: g.tensor_tensor(out=Mt[s_][:], in0=Mt[s_][:],
                                                                               in1=maskm[:], op=ALU.mult),
                                      [r_const, r_dec[s_]], [r_dec[s_]])
                            if (not is_ret) and (ui == 0 or not DIFF_NEW):
                                tk.op("act", lambda a, s_=s_, lg=lg, bias=bias: a.activation(
                                    out=Ft[s_][:], in_=dfull[:], func=AF.Exp, scale=lg, bias=bias),
                                    [r_const], [r_dec[s_]])
                            if is_ret:
                                tk.op("act", lambda a, s_=s_, lg=lg, bias=bias: a.activation(
                                    out=tabT[s_][:], in_=cdt[:], func=AF.Exp, scale=-lg, bias=bias),
                                    [r_const], [r_tab])
                                tk.op("dve", lambda v, s_=s_, lg=lg: v.memset(lgcol[64 * s_:64 * s_ + 64, :], lg),
                                      [], [r_tab])
                        if is_ret:
                            tk.op("act", lambda a: a.activation(out=Bt[:], in_=cqi[:], func=AF.Exp,
                                                                scale=lgcol[:, 0:1]),
                                  [r_const, r_tab], [r_tab])
                        elif ui > 0:
                            tk.op("dve", lambda v: v.tensor_scalar(out=tabT[0][:], in0=cdt[:],
                                                                   scalar1=float(alibi_slope(ui)), scalar2=None,
                                                                   op0=ALU.mult),
                                  [r_const], [r_tab])
                        if is_ret:
                            qc, kc_, vc, gc = OFF_RQ + 128 * ui, OFF_RK + 128 * ui, OFF_RV + 256 * ui, OFF_RG + 256 * ui
                        else:
                            qc, kc_, vc = OFF_DQ + 128 * ui, OFF_DK + 128 * ui, OFF_DV + 128 * ui
                        slab, rsl = load_slab(wcols(w_in[l], qc), key=nkey())
                        for t5 in range(NT5):
                            if is_ret:
                                def evq(b, t5=t5):
                                    tk.op("act", lambda a: a.activation(out=QT[:, t5 * 512:(t5 + 1) * 512],
                                                                        in_=banks[b][:, :], func=AF.Copy),
                                          [rbank[b]], [r_q[t5]])
                                    tk.op("dve", lambda v: v.tensor_tensor(out=QS[:, t5 * 512:(t5 + 1) * 512],
                                                                           in0=banks[b][:, :], in1=Bt[:], op=ALU.mult),
                                          [r_tab], [rbank[b], r_qs[t5]])
                                proj_fm(slab, rsl, t5, evq)
                            else:
                                proj_fm(slab, rsl, t5, evac_copy(QT[:, t5 * 512:(t5 + 1) * 512], r_q[t5]))
                        flush()
                        slab, rsl = load_slab(wcols(w_in[l], kc_), key=nkey())
                        for t5 in range(NT5):
                            proj_fm(slab, rsl, t5, evac_copy(KT[:, t5 * 512:(t5 + 1) * 512], r_k[t5]))
                        for hh in range(2 if is_ret else 1):
                            slab, rsl = load_slab(wcols(w_in[l], vc + 128 * hh), key=nkey())
                            proj_v(slab, rsl, hh)
                        if is_ret:
                            for hh in range(2):
                                slab, rsl = load_slab(wcols(w_in[l], gc + 128 * hh), key=nkey())
                                for t5 in range(NT5):
                                    def ev(b, hh=hh, t5=t5):
                                        tk.op("act", lambda a: a.activation(
                                            out=SG[:, hh, t5 * 512:(t5 + 1) * 512], in_=banks[b][:, :], func=AF.Silu),
                                            [rbank[b]], [r_sg[t5]])
                                    proj_fm(slab, rsl, t5, ev)

                        ZB = [6, 7]
                        for t5 in range(NT5):
                            OB = [6, 7] if (is_ret and t5 % 2 == 1) else [4, 5]
                            nkb = 4 * (t5 + 1)
                            kb0 = max(0, 4 * t5 - 4) if ((not is_ret) and ui == 0) else 0
                            ring = {"i": 0}

                            def issue_S(b_):
                                sb_ = [(ring["i"] % 2) * 2, (ring["i"] % 2) * 2 + 1]
                                ring["i"] += 1
                                offd = (512 * t5 - 128 * b_) >= 128
                                Qsrc, rq_ = (QS, r_qs[t5]) if (is_ret and offd and RET_NEW) else (QT, r_q[t5])
                                for s_ in range(2):
                                    bk = sb_[s_]
                                    tk.op("pe", lambda t, bk=bk, s_=s_: t.matmul(
                                        banks[bk][:, :], KT[64 * s_:64 * s_ + 64, b_ * 128:(b_ + 1) * 128],
                                        Qsrc[64 * s_:64 * s_ + 64, t5 * 512:(t5 + 1) * 512], start=True, stop=True),
                                        [r_k[b_ // 4], rq_], [rbank[bk]])
                                return sb_

                            def make_P(b_, sb_):
                                delta = 512 * t5 - 128 * b_
                                offd = delta >= 128
                                m = delta // 128 + 3
                                ps = []
                                for s_ in range(2):
                                    ds_ = s_ if is_ret else 0
                                    pi = ectr["P"] % 6
                                    ectr["P"] += 1
                                    bk = sb_[s_]
                                    if is_ret:
                                        if offd and not RET_NEW:
                                            c = ret_gamma(2 * ui + s_) ** delta
                                            tk.op("dve", lambda v, bk=bk, pi=pi, c=c: v.scalar_tensor_tensor(
                                                out=Pb[pi][:], in0=banks[bk][:, :], scalar=float(c), in1=Ft[0][:, :],
                                                op0=ALU.mult, op1=ALU.mult),
                                                [rbank[bk], r_dec[0]], [r_P[pi]])
                                        elif offd:
                                            tk.op("act", lambda a, bk=bk, pi=pi, s_=s_: a.activation(
                                                out=Pb[pi][:], in_=banks[bk][:, :], func=AF.Copy,
                                                scale=tabT[s_][:, m:m + 1]),
                                                [rbank[bk], r_tab], [r_P[pi]])
                                        else:
                                            tile_ap = Mt[ds_][:, 384 + delta:384 + delta + 512]
                                            tk.op("dve", lambda v, bk=bk, pi=pi, tile_ap=tile_ap: v.tensor_tensor(
                                                out=Pb[pi][:], in0=banks[bk][:, :], in1=tile_ap, op=ALU.mult),
                                                [rbank[bk], r_dec[ds_]], [r_P[pi]])
                                    elif ui == 0 or not DIFF_NEW:
                                        tile_ap = Ft[0][:, :] if offd else Mt[0][:, 384 + delta:384 + delta + 512]
                                        cb = -alibi_slope(ui) * delta if offd else 0.0
                                        ei = ectr["E"] % 4
                                        ectr["E"] += 1
                                        tk.op("act", lambda a, bk=bk, ei=ei, cb=cb: a.activation(
                                            out=Eb[ei][:], in_=banks[bk][:, :], func=AF.Exp, scale=0.125, bias=float(cb)),
                                            [rbank[bk]], [r_E[ei]])
                                        tk.op("dve", lambda v, ei=ei, pi=pi, tile_ap=tile_ap: v.tensor_tensor(
                                            out=Pb[pi][:], in0=Eb[ei][:], in1=tile_ap, op=ALU.mult),
                                            [r_E[ei], r_dec[0]], [r_P[pi]])
                                    else:
                                        if offd:
                                            tk.op("act", lambda a, bk=bk, pi=pi: a.activation(
                                                out=Pb[pi][:], in_=banks[bk][:, :], func=AF.Exp, scale=0.125,
                                                bias=tabT[0][:, m:m + 1]),
                                                [rbank[bk], r_tab], [r_P[pi]])
                                        else:
                                            ei = ectr["E"] % 4
                                            ectr["E"] += 1
                                            tk.op("act", lambda a, bk=bk, ei=ei: a.activation(
                                                out=Eb[ei][:], in_=banks[bk][:, :], func=AF.Exp, scale=0.125,
                                                bias=tabT[0][:, m:m + 1]),
                                                [rbank[bk], r_tab], [r_E[ei]])
                                            mk = maskm[:, 384 + delta:384 + delta + 512]
                                            tk.op("dve", lambda v, ei=ei, pi=pi, mk=mk: v.tensor_tensor(
                                                out=Pb[pi][:], in0=Eb[ei][:], in1=mk, op=ALU.mult),
                                                [r_E[ei], r_const], [r_P[pi]])
                                    ps.append(pi)
                                return ps

                            def issue_PV(b_, ps):
                                first, last = b_ == kb0, b_ == nkb - 1
                                for s_ in range(2):
                                    pi = ps[s_]
                                    vh = s_ if is_ret else 0
                                    if not is_ret:
                                        tk.op("pe", lambda t, s_=s_, pi=pi: t.matmul(
                                            banks[ZB[s_]][:, :], ones_b[:, :], Pb[pi][:, :], start=first, stop=last),
                                            [r_P[pi], r_const], [rbank[ZB[s_]]])
                                    tk.op("pe", lambda t, s_=s_, pi=pi, vh=vh: t.matmul(
                                        banks[OB[s_]][:, :], Vt[:, b_, vh, :], Pb[pi][:, :], start=first, stop=last),
                                        [r_P[pi], r_v[b_ // 4]], [rbank[OB[s_]]])

                            prev = None
                            for b_ in range(kb0, nkb):
                                sb_ = issue_S(b_)
                                if prev is not None:
                                    issue_PV(*prev)
                                ps = make_P(b_, sb_)
                                prev = (b_, ps)
                                if b_ == kb0 + 1:
                                    flush()
                            issue_PV(*prev)

                            qs = slice(t5 * 512, (t5 + 1) * 512)
                            tbs = r_mix[t5 * 4:t5 * 4 + 4]
                            if is_ret:
                                for s_ in range(2):
                                    ob = OB[s_]
                                    e0 = ep[3 * s_]
                                    tk.op("act", lambda a, ob=ob, e0=e0: a.activation(out=e0[:], in_=banks[ob][:, :],
                                                                                      func=AF.Square),
                                          [rbank[ob]], [r_ep[3 * s_]])
                                    pending.append(ret_part2(s_, 2 * ui + s_, ob, qs, tbs, r_sg[t5], l))
                            else:
                                e0, e1, e2, e3, e4, e5 = ep
                                tk.op("dve", lambda v: v.tensor_copy(out=e2[:], in_=banks[OB[0]][:, :]),
                                      [rbank[OB[0]]], [r_ep[2]])
                                tk.op("dve", lambda v: v.tensor_copy(out=e3[:], in_=banks[OB[1]][:, :]),
                                      [rbank[OB[1]]], [r_ep[3]])
                                tk.op("act", lambda a: a.activation(out=e0[:], in_=banks[ZB[0]][:, :], func=AF.Ln),
                                      [rbank[ZB[0]]], [r_ep[0]])
                                tk.op("act", lambda a: a.activation(out=e1[:], in_=banks[ZB[1]][:, :], func=AF.Ln),
                                      [rbank[ZB[1]]], [r_ep[1]])
                                tk.op("act", lambda a: a.activation(out=e0[:], in_=e0[:], func=AF.Exp, scale=-1.0),
                                      [r_ep[0]], [r_ep[0]])
                                tk.op("act", lambda a: a.activation(out=e1[:], in_=e1[:], func=AF.Exp, scale=-1.0),
                                      [r_ep[1]], [r_ep[1]])
                                tk.op("dve", lambda v: v.tensor_tensor(out=e2[:], in0=e2[:], in1=e0[:], op=ALU.mult),
                                      [r_ep[0]], [r_ep[2]])
                                tk.op("dve", lambda v: v.tensor_tensor(out=e3[:], in0=e3[:], in1=e1[:], op=ALU.mult),
                                      [r_ep[1]], [r_ep[3]])
                                tk.op("dve", lambda g: g.scalar_tensor_tensor(
                                    out=e4[:], in0=e3[:], scalar=neglam[:, l:l + 1], in1=e2[:],
                                    op0=ALU.mult, op1=ALU.add),
                                    [r_ep[2], r_ep[3], r_const], [r_ep[4]])
                                tk.op("act", lambda a: a.activation(out=e5[:], in_=e4[:], func=AF.Square),
                                      [r_ep[4]], [r_ep[5]])
                                pending.append(diff_part2(4 + ui, qs, tbs, l))
                    flush()

                    tk.barrier()
                    with ExitStack() as oo:
                        Wo = oo.enter_context(nc.sbuf_tensor("Wo_%d_%d" % (seq, l), [128, 8, D], BF16))
                        r_wo = R()
                        wo_flat = Wo[:].rearrange("p a b -> p (a b)")
                        if seq == 0:
                            for kc in range(8):
                                st, rst = stage(w_out[l][kc * 128:(kc + 1) * 128, :])
                                cast("act" if kc % 2 == 0 else "dve", Wo[:, kc, :], st[:], [rst], [r_wo])
                            tk.dma(woscr[l], wo_flat, [r_wo], [], d_big)
                        else:
                            tk.dma(wo_flat, woscr[l], [], [r_wo], d_big)
                        tk.dma(gbc[:], ffn_bc[l], [], [r_gbc], d_gbc)
                        for tb in range(NTB):
                            i = ctr["hb"] % NHB
                            ctr["hb"] += 1
                            tk.dma(hb[i][:], blk(hsrc, tb), [], [r_hb[i]], d_hb[i])
                            for nh in range(2):
                                b = next_bank()

                                def mm(t, b=b, nh=nh):
                                    ins = None
                                    for kc in range(8):
                                        ins = t.matmul(banks[b][:, :], mixT[:, kc, tb * 128:(tb + 1) * 128],
                                                       Wo[:, kc, nh * 512:(nh + 1) * 512], start=(kc == 0), stop=(kc == 7))
                                    return ins
                                tk.op("pe", mm, [r_wo, r_mix[tb]], [rbank[b]])
                                tk.op("dve", lambda v, b=b, nh=nh: v.tensor_tensor(
                                    out=hb[i][:, nh * 512:(nh + 1) * 512], in0=hb[i][:, nh * 512:(nh + 1) * 512],
                                    in1=banks[b][:, :], op=ALU.add),
                                    [rbank[b], r_hb[i]], [r_hb[i]])
                            tk.dma(blk(y[seq], tb), hb[i][:], [r_hb[i]], [], d_hst[i])
                            fused_a(i, tb)
                            if tb > 1:
                                stage_b(tb - 2)
                        stage_b(NTB - 2)
                        stage_b(NTB - 1)
                        tk.barrier()
                    tk.barrier()

                with ExitStack() as ff:
                    HT = S // 2
                    gT = ff.enter_context(nc.sbuf_tensor("gT_%d_%d" % (seq, l), [128, NJ, HT], BF16))
                    r_g = [R() for _ in range(NTB)]
                    Wd = ff.enter_context(nc.sbuf_tensor("Wd_%d_%d" % (seq, l), [128, NJ, D], BF16))
                    r_wd = R()
                    sgb = [ff.enter_context(nc.sbuf_tensor("sgb%d_%d_%d" % (i, seq, l), [128, 512], F32))
                           for i in range(2)]
                    r_sgb = [R(), R()]
                    k_sg = 0
                    last = (l == depth - 1)
                    if last:
                        tk.dma(gbc[:], final_bc[:, :], [], [r_gbc], d_gbc)
                    else:
                        tk.dma(gbc[:], attn_bc[l + 1], [], [r_gbc], d_gbc)
                    wd_flat = Wd[:].rearrange("p a b -> p (a b)")
                    if seq > 0:
                        tk.dma(wd_flat, wdscr[l], [], [r_wd], d_big)
                    for hf in range(2):
                        if hf == 1 and seq == 0:
                            tk.dma(wdscr[l], wd_flat, [r_wd], [], d_big)
                        for j in range(NJ):
                            slg, rg_ = load_slab(wcols(w_gate[l], j * 128), "act", key=l * NSLAB + 24 + j)
                            slu, ru_ = load_slab(wcols(w_up[l], j * 128), "dve", key=l * NSLAB + 46 + j)
                            if hf == 0 and seq == 0:
                                st, rst = stage(w_down[l][j * 128:(j + 1) * 128, :])
                                cast("pool", Wd[:, j, :], st[:], [rst], [r_wd])
                            for t5 in (2 * hf, 2 * hf + 1):
                                bg = next_bank()
                                bu = next_bank()

                                def mmg(t, bb=bg, slab=slg, t5=t5):
                                    ins = None
                                    for kc in range(8):
                                        ins = t.matmul(banks[bb][:, :], slab[:, kc, :],
                                                       uT[:, kc, t5 * 512:(t5 + 1) * 512],
                                                       start=(kc == 0), stop=(kc == 7))
                                    return ins
                                tk.op("pe", mmg, [rg_] + r_uT[t5 * 4:t5 * 4 + 4], [rbank[bg]])

                                def mmu(t, bb=bu, slab=slu, t5=t5):
                                    ins = None
                                    for kc in range(8):
                                        ins = t.matmul(banks[bb][:, :], slab[:, kc, :],
                                                       uT[:, kc, t5 * 512:(t5 + 1) * 512],
                                                       start=(kc == 0), stop=(kc == 7))
                                    return ins
                                tk.op("pe", mmu, [ru_] + r_uT[t5 * 4:t5 * 4 + 4], [rbank[bu]])
                                si = k_sg % 2
                                k_sg += 1
                                tk.op("act", lambda a, si=si, bg=bg: a.activation(out=sgb[si][:], in_=banks[bg][:, :],
                                                                                  func=AF.Silu),
                                      [rbank[bg]], [r_sgb[si]])
                                lo = (t5 - 2 * hf) * 512
                                tk.op("dve", lambda v, si=si, bu=bu, j=j, lo=lo: v.tensor_tensor(
                                    out=gT[:, j, lo:lo + 512], in0=sgb[si][:], in1=banks[bu][:, :], op=ALU.mult),
                                    [r_sgb[si], rbank[bu]], r_g[t5 * 4:t5 * 4 + 4])
                        for tb in range(8 * hf, 8 * hf + 8):
                            i = ctr["hb"] % NHB
                            ctr["hb"] += 1
                            tk.dma(hb[i][:], blk(y[seq], tb), [], [r_hb[i]], d_hb[i])
                            lo = (tb - 8 * hf) * 128
                            for nh in range(2):
                                b = next_bank()

                                def mm(t, b=b, nh=nh, lo=lo):
                                    ins = None
                                    for j in range(NJ):
                                        ins = t.matmul(banks[b][:, :], gT[:, j, lo:lo + 128],
                                                       Wd[:, j, nh * 512:(nh + 1) * 512],
                                                       start=(j == 0), stop=(j == NJ - 1))
                                    return ins
                                tk.op("pe", mm, [r_wd, r_g[tb]], [rbank[b]])
                                tk.op("dve", lambda v, b=b, nh=nh: v.tensor_tensor(
                                    out=hb[i][:, nh * 512:(nh + 1) * 512], in0=hb[i][:, nh * 512:(nh + 1) * 512],
                                    in1=banks[b][:, :], op=ALU.add),
                                    [rbank[b], r_hb[i]], [r_hb[i]])
                            if last:
                                norm_stats(i, tb)
                                tk.op("dve", lambda v: v.scalar_tensor_tensor(
                                    out=hb[i][:], in0=hb[i][:], scalar=stat[:, 2 * NTB + tb:2 * NTB + tb + 1],
                                    in1=gbc[:], op0=ALU.mult, op1=ALU.mult),
                                    [r_hb[i], r_stat[tb], r_gbc], [r_hb[i]])
                            tok = tk.dma(blk(y[seq], tb), hb[i][:], [r_hb[i]], [], d_hst[i])
                            if last:
                                out_tokens.append(tok)
                            else:
                                fused_a(i, tb)
                                if tb > 8 * hf + 1:
                                    stage_b(tb - 2)
                        if not last:
                            stage_b(8 * hf + 6)
                            stage_b(8 * hf + 7)
                    tk.barrier()
                tk.barrier()
    tk.finish(out_tokens)
    tk.barrier()
    es.close()
    return nc


def host_consts():
    p = np.arange(128, dtype=np.float32)[:, None]
    c = np.arange(896, dtype=np.float32)[None, :]
    dm = (c - 384.0 - p).astype(np.float32)
    df = (np.arange(512, dtype=np.float32)[None, :] - p).astype(np.float32)
    ident = np.eye(128, dtype=np.float32)
    dt_ = (p - 128.0 * (np.arange(16, dtype=np.float32)[None, :] - 3.0)).astype(np.float32)
    qi = np.ascontiguousarray(np.broadcast_to(np.arange(512, dtype=np.float32)[None, :], (128, 512)))
    return ident, dm, df, dt_, qi


_CACHE = {}


def run(inputs, nseq, depth, ncores, trace=False):
    key = (nseq, depth)
    if key not in _CACHE:
        _CACHE[key] = build_program(nseq, depth)
    nc = _CACHE[key]
    f = lambda a: np.ascontiguousarray(np.asarray(a, dtype=np.float32))
    ident, dm, df, dt_, qi_ = host_consts()
    bc = lambda a: np.ascontiguousarray(np.broadcast_to(f(a)[:, None, :], (a.shape[0], 128, a.shape[1])))
    lam = np.concatenate([f(inputs["lambda_q1"])[:, None, :], f(inputs["lambda_k1"])[:, None, :],
                          f(inputs["lambda_q2"])[:, None, :], f(inputs["lambda_k2"])[:, None, :]], axis=1)
    lam = lam[:depth].reshape(1, depth * 4 * 64)
    shared = {
        "w_in": f(inputs["w_in"])[:depth], "w_out": f(inputs["w_out"])[:depth],
        "w_gate": f(inputs["w_gate"])[:depth], "w_up": f(inputs["w_up"])[:depth],
        "w_down": f(inputs["w_down"])[:depth],
        "attn_bc": bc(f(inputs["attn_norm"])[:depth]), "ffn_bc": bc(f(inputs["ffn_norm"])[:depth]),
        "final_bc": np.ascontiguousarray(np.broadcast_to(f(inputs["final_norm"])[None, :], (128, D))),
        "retn_col": np.ascontiguousarray(f(inputs["ret_norm"])[:depth].T),
        "difn_col": np.ascontiguousarray(f(inputs["diff_norm"])[:depth].T),
        "lam_in": np.ascontiguousarray(np.broadcast_to(lam, (128, depth * 4 * 64))),
        "c_ident": ident, "c_dm": dm, "c_df": df, "c_dt": dt_, "c_qi": qi_,
    }
    xs = f(inputs["x"])
    in_maps = []
    for c in range(ncores):
        m = dict(shared)
        m["x"] = np.ascontiguousarray(xs[c * nseq:(c + 1) * nseq])
        in_maps.append(m)
    res = run_bass_kernel_spmd(nc, in_maps, core_ids=list(range(ncores)), **({"trace": True} if trace else {}))
    out = np.concatenate([r["y"] for r in res.results], axis=0)
    return out, res


def kernel(x, attn_norm, w_in, ret_norm, lambda_q1, lambda_k1, lambda_q2, lambda_k2,
           diff_norm, w_out, ffn_norm, w_gate, w_up, w_down, final_norm):
    inputs = dict(x=x, attn_norm=attn_norm, w_in=w_in, ret_norm=ret_norm, lambda_q1=lambda_q1,
                  lambda_k1=lambda_k1, lambda_q2=lambda_q2, lambda_k2=lambda_k2, diff_norm=diff_norm,
                  w_out=w_out, ffn_norm=ffn_norm, w_gate=w_gate, w_up=w_up, w_down=w_down,
                  final_norm=final_norm)
    out, _ = run(inputs, BATCH // NCORES, DEPTH, NCORES)
    return out.astype(np.float32)
```

```python
import math
import os
from contextlib import ExitStack

import numpy as np
import concourse.bass as bass
import concourse.mybir as mybir
from concourse.bass_utils import run_bass_kernel_spmd

F32 = mybir.dt.float32
BF16 = mybir.dt.bfloat16
AF = mybir.ActivationFunctionType
ALU = mybir.AluOpType
AX = mybir.AxisListType

D = 1024
S = 2048
DEPTH = 2
BATCH = 32
NCORES = 8
FF = 2816
NJ = FF // 128
INW = 3072
EPS = 1e-6
OFF_RQ, OFF_RK, OFF_RV, OFF_RG, OFF_DQ, OFF_DK, OFF_DV = 0, 256, 512, 1024, 1536, 2048, 2560
NTB = S // 128
NT5 = S // 512


class R:
    __slots__ = ("w", "rs")

    def __init__(self):
        self.w = None
        self.rs = {}


class Eng:
    def __init__(self, name, handle):
        self.name = name
        self.h = handle
        self.sem = None
        self.count = 0
        self.known = {}


class DSem:
    def __init__(self, sem):
        self.sem = sem
        self.count = 0


class Tracker:
    def __init__(self, nc, es):
        self.nc = nc
        self.es = es
        self.nsem = 0
        self.engs = {
            "pe": Eng("pe", nc.tensor),
            "act": Eng("act", nc.scalar),
            "dve": Eng("dve", nc.vector),
            "pool": Eng("pool", nc.gpsimd),
            "sp": Eng("sp", nc.sync),
        }
        self.dsems = []
        self.epoch()

    def new_sem(self):
        self.nsem += 1
        return self.es.enter_context(self.nc.semaphore("ts%d" % self.nsem))

    def epoch(self):
        for e in self.engs.values():
            if e.name == "sp":
                continue
            e.sem = self.new_sem()
            e.count = 0

    def dsem(self):
        d = DSem(self.new_sem())
        self.dsems.append(d)
        return d

    def _waits(self, e, r, w):
        waits = {}

        def need(tok):
            if tok is None:
                return
            sem, val = tok
            if e.name == "pe" and sem is e.sem:
                return
            if e.known.get(sem, 0) >= val:
                return
            if waits.get(sem, (None, 0))[1] < val:
                waits[sem] = (sem, val)

        for x in r:
            need(x.w)
        for x in w:
            need(x.w)
            for t in x.rs.values():
                need(t)
        for sem, val in waits.values():
            e.known[sem] = val
            e.h.wait_ge(sem, val)

    @staticmethod
    def _mark(tok, r, w):
        for x in r:
            old = x.rs.get(tok[0])
            if old is None or old[1] < tok[1]:
                x.rs[tok[0]] = tok
        for x in w:
            x.w = tok
            x.rs = {}

    def op(self, eng, fn, r=(), w=()):
        e = self.engs[eng]
        self._waits(e, r, w)
        ins = fn(e.h)
        e.count += 1
        tok = (e.sem, e.count)
        ins.then_inc(e.sem, 1)
        self._mark(tok, r, w)
        return tok

    def dma(self, out, in_, r, w, ds):
        e = self.engs["sp"]
        self._waits(e, r, w)
        ins = e.h.dma_start(out=out, in_=in_)
        ds.count += 16
        tok = (ds.sem, ds.count)
        ins.then_inc(ds.sem, 16)
        self._mark(tok, r, w)
        return tok

    def barrier(self):
        toks = []
        for e in self.engs.values():
            if e.name != "sp" and e.count > 0:
                toks.append((e.sem, e.count))
        for d in self.dsems:
            if d.count > 0:
                toks.append((d.sem, d.count))
        for e in self.engs.values():
            for sem, val in toks:
                if sem is e.sem:
                    continue
                if e.known.get(sem, 0) >= val:
                    continue
                e.known[sem] = val
                e.h.wait_ge(sem, val)

    def finish(self, toks):
        e = self.engs["sp"]
        for sem, val in toks:
            if e.known.get(sem, 0) >= val:
                continue
            e.known[sem] = val
            e.h.wait_ge(sem, val)


def ret_gamma(h):
    return 1.0 - 2.0 ** (-5.0 - h)


def alibi_slope(h):
    return 2.0 ** (-8.0 * (h + 1) / 4.0)


def build_program(nseq, depth):
    nc = bass.Bass("TRN2", target_bir_lowering=False)
    es = ExitStack()

    def dram(name, shape, kind="ExternalInput", dt=F32):
        return nc.dram_tensor(name, list(shape), dt, kind=kind).ap()

    x = dram("x", [nseq, S, D])
    y = dram("y", [nseq, S, D], kind="ExternalOutput")
    w_in = dram("w_in", [depth, D, INW])
    w_out = dram("w_out", [depth, D, D])
    w_gate = dram("w_gate", [depth, D, FF])
    w_up = dram("w_up", [depth, D, FF])
    w_down = dram("w_down", [depth, FF, D])
    attn_bc = dram("attn_bc", [depth, 128, D])
    ffn_bc = dram("ffn_bc", [depth, 128, D])
    final_bc = dram("final_bc", [128, D])
    retn_col = dram("retn_col", [128, depth])
    difn_col = dram("difn_col", [128, depth])
    lam_in = dram("lam_in", [128, depth * 4 * 64])
    c_ident = dram("c_ident", [128, 128])
    c_dm = dram("c_dm", [128, 896])
    c_df = dram("c_df", [128, 512])
    c_dt = dram("c_dt", [128, 16])
    c_qi = dram("c_qi", [128, 512])

    tk = Tracker(nc, es)

    def sb(name, shape, dt):
        return es.enter_context(nc.sbuf_tensor(name, list(shape), dt))

    ident = sb("ident", [128, 128], F32)
    ones_f = sb("ones_f", [128, 128], F32)
    ones_b = sb("ones_b", [128, 128], BF16)
    dmc = sb("dmc", [128, 896], F32)
    maskm = sb("maskm", [128, 896], F32)
    dfull = sb("dfull", [128, 512], F32)
    cdt = sb("cdt", [128, 16], F32)
    cqi = sb("cqi", [128, 512], F32)
    retn = sb("retn", [128, depth], F32)
    difn = sb("difn", [128, depth], F32)
    lamt = sb("lamt", [128, depth * 4 * 64], F32)
    lamw = sb("lamw", [128, 2 * 64], F32)
    lams = sb("lams", [128, 8], F32)
    neglam = sb("neglam", [128, depth], F32)
    r_const = R()
    r_lam = R()
    cds = tk.dsem()
    tk.dma(ident[:], c_ident[:, :], [], [r_const], cds)
    tk.dma(dmc[:], c_dm[:, :], [], [r_const], cds)
    tk.dma(dfull[:], c_df[:, :], [], [r_const], cds)
    tk.dma(cdt[:], c_dt[:, :], [], [r_const], cds)
    tk.dma(cqi[:], c_qi[:, :], [], [r_const], cds)
    tk.dma(retn[:], retn_col[:, :], [], [r_const], cds)
    tk.dma(difn[:], difn_col[:, :], [], [r_const], cds)
    tk.dma(lamt[:], lam_in[:, :], [], [r_const], cds)
    r_const.w = (cds.sem, cds.count)
    tk.op("dve", lambda v: v.memset(ones_f[:], 1.0), [], [r_const])
    tk.op("dve", lambda v: v.memset(ones_b[:], 1.0), [], [r_const])
    tk.op("dve", lambda v: v.tensor_single_scalar(out=maskm[:], in_=dmc[:], scalar=0.0, op=ALU.is_ge),
          [r_const], [r_const])
    tk.op("dve", lambda v: v.tensor_scalar_max(out=dmc[:], in0=dmc[:], scalar1=0.0), [r_const], [r_const])
    for l in range(depth):
        base = l * 256
        tk.op("dve", lambda v, b=base: v.tensor_tensor(out=lamw[:, 0:64], in0=lamt[:, b:b + 64],
                                                       in1=lamt[:, b + 64:b + 128], op=ALU.mult),
              [r_const], [r_lam])
        tk.op("dve", lambda v, b=base: v.tensor_tensor(out=lamw[:, 64:128], in0=lamt[:, b + 128:b + 192],
                                                       in1=lamt[:, b + 192:b + 256], op=ALU.mult),
              [r_const], [r_lam])
        tk.op("dve", lambda v: v.reduce_sum(out=lams[:, 0:1], in_=lamw[:, 0:64], axis=AX.X), [r_lam], [r_lam])
        tk.op("dve", lambda v: v.reduce_sum(out=lams[:, 1:2], in_=lamw[:, 64:128], axis=AX.X), [r_lam], [r_lam])
        tk.op("act", lambda a: a.activation(out=lams[:, 2:4], in_=lams[:, 0:2], func=AF.Exp), [r_lam], [r_lam])
        lam_init = 0.8 - 0.6 * math.exp(-0.3 * l)
        tk.op("dve", lambda v, li=lam_init: v.tensor_scalar(out=lams[:, 4:5], in0=lams[:, 3:4], scalar1=-li,
                                                            scalar2=None, op0=ALU.add), [r_lam], [r_lam])
        tk.op("dve", lambda v, l=l: v.tensor_tensor(out=neglam[:, l:l + 1], in0=lams[:, 4:5], in1=lams[:, 2:3],
                                                    op=ALU.subtract), [r_lam], [r_const])
        tk.op("dve", lambda v, l=l, li=lam_init: v.tensor_scalar(out=difn[:, l:l + 1], in0=difn[:, l:l + 1],
                                                                 scalar1=1.0 - li, scalar2=None, op0=ALU.mult),
              [r_const], [r_const])

    banks = [es.enter_context(nc.psum_tensor("bank%d" % i, [128, 512], F32)) for i in range(8)]
    rbank = [R() for _ in range(8)]

    NSTG, NWS = 2, 4
    stg = [sb("stg%d" % i, [128, 1024], F32) for i in range(NSTG)]
    r_stg = [R() for _ in range(NSTG)]
    d_stg = [tk.dsem() for _ in range(NSTG)]
    wsl = [sb("wsl%d" % i, [128, 1024], BF16) for i in range(NWS)]
    r_wsl = [R() for _ in range(NWS)]
    ctr = {"stg": 0, "wsl": 0, "bank": 0, "hb": 0, "ev": 0}

    def stage(src_ap, view3=None):
        i = ctr["stg"] % NSTG
        ctr["stg"] += 1
        dst = stg[i][:] if view3 is None else stg[i][:].rearrange("p (a b) -> p a b", a=view3[0], b=view3[1])
        tk.dma(dst, src_ap, [], [r_stg[i]], d_stg[i])
        return stg[i], r_stg[i]

    def cast(eng, out_ap, in_ap, r, w):
        if eng == "act":
            tk.op("act", lambda a: a.activation(out=out_ap, in_=in_ap, func=AF.Copy), r, w)
        else:
            tk.op(eng, lambda g: g.tensor_copy(out=out_ap, in_=in_ap), r, w)

    NSLAB = 68
    wscr = nc.dram_tensor("wscr", [depth * NSLAB, 128, 1024], BF16).ap()
    woscr = nc.dram_tensor("woscr", [depth, 128, 8 * D], BF16).ap()
    wdscr = nc.dram_tensor("wdscr", [depth, 128, NJ * D], BF16).ap()
    d_wsl = [tk.dsem() for _ in range(NWS)]
    d_wst = [tk.dsem() for _ in range(NWS)]
    d_big = tk.dsem()
    cur = {"seq": 0}

    def load_slab(src_ap, eng="pool", key=None):
        j = ctr["wsl"] % NWS
        ctr["wsl"] += 1
        if cur["seq"] == 0:
            st, rst = stage(src_ap, (8, 128))
            cast(eng, wsl[j][:], st[:], [rst], [r_wsl[j]])
            tk.dma(wscr[key], wsl[j][:], [r_wsl[j]], [], d_wst[j])
        else:
            tk.dma(wsl[j][:], wscr[key], [], [r_wsl[j]], d_wsl[j])
        return wsl[j][:].rearrange("p (a b) -> p a b", a=8, b=128), r_wsl[j]

    def wcols(w_ap_l, c0):
        return w_ap_l.rearrange("(kc p) n -> p kc n", p=128)[:, :, c0:c0 + 128]

    def next_bank(lo=0, hi=8):
        i = lo + ctr["bank"] % (hi - lo)
        ctr["bank"] += 1
        return i

    NHB = 3
    hb = [sb("hb%d" % i, [128, D], F32) for i in range(NHB)]
    r_hb = [R() for _ in range(NHB)]
    d_hb = [tk.dsem() for _ in range(NHB)]
    d_hst = [tk.dsem() for _ in range(NHB)]
    NUN = 3
    un = [sb("un%d" % i, [128, D], F32) for i in range(NUN)]
    r_un = [R() for _ in range(NUN)]
    junk = sb("junk", [128, D], BF16)
    r_junk = R()
    gbc = sb("gbc", [128, D], F32)
    r_gbc = R()
    d_gbc = tk.dsem()
    stat = sb("stat", [128, 3 * NTB], F32)
    r_stat = [R() for _ in range(NTB)]

    def blk(ap_seq, tb):
        return ap_seq.rearrange("(tb p) d -> p tb d", p=128)[:, tb, :]

    out_tokens = []

    def norm_stats(i, tb):
        tk.op("act", lambda a: a.activation(out=junk[:], in_=hb[i][:], func=AF.Square,
                                            accum_out=stat[:, tb:tb + 1]),
              [r_hb[i]], [r_junk, r_stat[tb]])
        tk.op("act", lambda a: a.activation(out=stat[:, NTB + tb:NTB + tb + 1], in_=stat[:, tb:tb + 1],
                                            func=AF.Sqrt, scale=1.0 / D, bias=EPS),
              [r_stat[tb]], [r_stat[tb]])
        tk.op("dve", lambda v: v.reciprocal(out=stat[:, 2 * NTB + tb:2 * NTB + tb + 1],
                                            in_=stat[:, NTB + tb:NTB + tb + 1]),
              [r_stat[tb]], [r_stat[tb]])

    uT = sb("uT", [128, 8, S], BF16)
    r_uT = [R() for _ in range(NTB)]

    for seq in range(nseq):
        cur["seq"] = seq
        for l in range(depth):
            skey = {"i": l * NSLAB}

            def nkey():
                skey["i"] += 1
                return skey["i"] - 1
            tk.barrier()
            tk.epoch()
            hsrc = x[seq] if l == 0 else y[seq]
            with ExitStack() as sl:

                def norm_phase(src, g_dram):
                    tk.dma(gbc[:], g_dram, [], [r_gbc], d_gbc)

                    def stage_a(tb):
                        i = ctr["hb"] % NHB
                        ctr["hb"] += 1
                        tk.dma(hb[i][:], blk(src, tb), [], [r_hb[i]], d_hb[i])
                        fused_a(i, tb)

                    stage_a(0)
                    stage_a(1)
                    for tb in range(NTB):
                        if tb + 2 < NTB:
                            stage_a(tb + 2)
                        stage_b(tb)

                if True:
                    def fused_a(i, tb):
                        norm_stats(i, tb)
                        u = tb % NUN
                        tk.op("dve", lambda v: v.scalar_tensor_tensor(
                            out=un[u][:], in0=hb[i][:], scalar=stat[:, 2 * NTB + tb:2 * NTB + tb + 1],
                            in1=gbc[:], op0=ALU.mult, op1=ALU.mult),
                            [r_hb[i], r_stat[tb], r_gbc], [r_un[u]])

                    def stage_b(tb):
                        u = tb % NUN
                        for half in range(2):
                            b = next_bank()

                            def tr(t, b=b, half=half):
                                ins = None
                                for q in range(4):
                                    kc = half * 4 + q
                                    ins = t.transpose(out=banks[b][:, q * 128:(q + 1) * 128],
                                                      in_=un[u][:, kc * 128:(kc + 1) * 128], identity=ident[:])
                                return ins
                            tk.op("pe", tr, [r_un[u], r_const], [rbank[b]])
                            src_ps = banks[b][:].rearrange("p (a b) -> p a b", a=4, b=128)
                            dst = uT[:, half * 4:half * 4 + 4, tb * 128:(tb + 1) * 128]
                            if half == 0:
                                tk.op("act", lambda a, s=src_ps, d=dst: a.activation(out=d, in_=s, func=AF.Copy),
                                      [rbank[b]], [r_uT[tb]])
                            else:
                                tk.op("dve", lambda v, s=src_ps, d=dst: v.tensor_copy(out=d, in_=s),
                                      [rbank[b]], [r_uT[tb]])

                if l == 0:
                    norm_phase(hsrc, attn_bc[l])

                with ExitStack() as att:
                    def asb(name, shape, dt):
                        return att.enter_context(nc.sbuf_tensor("%s_%d_%d" % (name, seq, l), list(shape), dt))
                    mixT = asb("mixT", [128, 8, S], BF16)
                    r_mix = [R() for _ in range(NTB)]
                    Wo = asb("Wo", [128, 8, D], BF16)
                    r_wo = R()
                    wo_flat = Wo[:].rearrange("p a b -> p (a b)")
                    if seq == 0:
                        for kc in range(8):
                            st, rst = stage(w_out[l][kc * 128:(kc + 1) * 128, :])
                            cast("act" if kc % 2 == 0 else "dve", Wo[:, kc, :], st[:], [rst], [r_wo])
                        tk.dma(woscr[l], wo_flat, [r_wo], [], d_big)
                    else:
                        tk.dma(wo_flat, woscr[l], [], [r_wo], d_big)
                    QT = asb("QT", [128, S], BF16)
                    QS = asb("QS", [128, S], BF16)
                    r_qs = [R() for _ in range(NT5)]
                    Bt = asb("Bt", [128, 512], F32)
                    lgcol = asb("lgcol", [128, 1], F32)
                    tabT = [asb("tabT%d" % s_, [128, 16], F32) for s_ in range(2)]
                    r_tab = R()
                    KT = asb("KT", [128, S], BF16)
                    Vt = asb("Vt", [128, NTB, 2, 128], BF16)
                    SG = asb("SG", [128, 2, S], BF16)
                    r_q = [R() for _ in range(NT5)]
                    r_k = [R() for _ in range(NT5)]
                    r_v = [R() for _ in range(NT5)]
                    r_sg = [R() for _ in range(NT5)]
                    Mt = [asb("Mt%d" % s_, [128, 896], F32) for s_ in range(2)]
                    Ft = [asb("Ft%d" % s_, [128, 512], F32) for s_ in range(1)]
                    r_dec = [R(), R()]
                    Eb = [asb("Eb%d" % i, [128, 512], F32) for i in range(4)]
                    r_E = [R() for _ in range(4)]
                    Pb = [asb("Pb%d" % i, [128, 512], BF16) for i in range(6)]
                    r_P = [R() for _ in range(6)]
                    ep = [asb("ep%d" % i, [128, 512], F32) for i in range(6)]
                    r_ep = [R() for _ in range(6)]
                    ectr = {"E": 0, "P": 0}
                    pending = []

                    def advance():
                        for ent in list(pending):
                            ent.pop(0)()
                            if not ent:
                                pending.remove(ent)

                    def flush():
                        while pending:
                            advance()

                    def ret_part2(s_, head, ob, qs, tbs, rsg, l_):
                        e0, e1, e2 = ep[3 * s_], ep[3 * s_ + 1], ep[3 * s_ + 2]
                        re0, re1, re2 = r_ep[3 * s_], r_ep[3 * s_ + 1], r_ep[3 * s_ + 2]
                        st_ = {}

                        def step_a():
                            zb = next_bank(0, 4)
                            tk.op("pe", lambda t: t.matmul(banks[zb][:, :], ones_f[:, :], e0[:], start=True, stop=True),
                                  [re0, r_const], [rbank[zb]])
                            tk.op("act", lambda a: a.activation(out=e1[:], in_=banks[zb][:, :], func=AF.Ln,
                                                                scale=1.0 / 128, bias=EPS),
                                  [rbank[zb]], [re1])

                        def step_b():
                            tk.op("act", lambda a: a.activation(out=e1[:], in_=e1[:], func=AF.Exp, scale=-0.5),
                                  [re1], [re1])

                        def step_c():
                            tk.op("dve", lambda v: v.scalar_tensor_tensor(
                                out=e2[:], in0=banks[ob][:, :], scalar=retn[:, l_:l_ + 1], in1=e1[:],
                                op0=ALU.mult, op1=ALU.mult),
                                [rbank[ob], re1, r_const], [re2])
                            tk.op("pool", lambda g: g.tensor_tensor(
                                out=mixT[:, head, qs], in0=e2[:], in1=SG[:, s_, qs], op=ALU.mult),
                                [re2, rsg], tbs)
                        return [step_a, step_b, step_c]

                    def diff_part2(head, qs, tbs, l_):
                        e0, e1, e2, e3, e4, e5 = ep
                        st_ = {}

                        def step_a():
                            tk.op("act", lambda a: a.activation(out=e0[:], in_=e0[:], func=AF.Exp, scale=-1.0),
                                  [r_ep[0]], [r_ep[0]])
                            tk.op("act", lambda a: a.activation(out=e1[:], in_=e1[:], func=AF.Exp, scale=-1.0),
                                  [r_ep[1]], [r_ep[1]])
                            tk.op("dve", lambda v: v.tensor_tensor(out=e2[:], in0=e2[:], in1=e0[:], op=ALU.mult),
                                  [r_ep[0]], [r_ep[2]])
                            tk.op("dve", lambda v: v.tensor_tensor(out=e3[:], in0=e3[:], in1=e1[:], op=ALU.mult),
                                  [r_ep[1]], [r_ep[3]])
                            tk.op("dve", lambda g: g.scalar_tensor_tensor(
                                out=e4[:], in0=e3[:], scalar=neglam[:, l_:l_ + 1], in1=e2[:],
                                op0=ALU.mult, op1=ALU.add),
                                [r_ep[2], r_ep[3], r_const], [r_ep[4]])

                        def step_n():
                            pass

                        def step_b1():
                            tk.op("act", lambda a: a.activation(out=e5[:], in_=e4[:], func=AF.Square),
                                  [r_ep[4]], [r_ep[5]])

                        def step_b():
                            zb = next_bank(0, 4)
                            tk.op("pe", lambda t: t.matmul(banks[zb][:, :], ones_f[:, :], e5[:], start=True, stop=True),
                                  [r_ep[5], r_const], [rbank[zb]])
                            tk.op("act", lambda a: a.activation(out=e0[:], in_=banks[zb][:, :], func=AF.Ln,
                                                                scale=1.0 / 128, bias=EPS),
                                  [rbank[zb]], [r_ep[0]])

                        def step_c():
                            tk.op("act", lambda a: a.activation(out=e0[:], in_=e0[:], func=AF.Exp, scale=-0.5),
                                  [r_ep[0]], [r_ep[0]])
                            tk.op("dve", lambda v: v.scalar_tensor_tensor(
                                out=mixT[:, head, qs], in0=e4[:], scalar=difn[:, l_:l_ + 1], in1=e0[:],
                                op0=ALU.mult, op1=ALU.mult),
                                [r_ep[4], r_ep[0], r_const], tbs)
                        return [step_a, step_n, step_b1, step_b, step_c]

                    def proj_fm(slab, rsl, t5, evac):
                        b = next_bank(0, 4)

                        def mm(t):
                            ins = None
                            for kc in range(8):
                                ins = t.matmul(banks[b][:, :], slab[:, kc, :], uT[:, kc, t5 * 512:(t5 + 1) * 512],
                                               start=(kc == 0), stop=(kc == 7))
                            return ins
                        tk.op("pe", mm, [rsl] + r_uT[t5 * 4:t5 * 4 + 4], [rbank[b]])
                        evac(b)

                    def evac_copy(dst_ap, rdst):
                        def f(b):
                            k = ctr["ev"]
                            ctr["ev"] += 1
                            if k % 2 == 0:
                                tk.op("act", lambda a: a.activation(out=dst_ap, in_=banks[b][:, :], func=AF.Copy),
                                      [rbank[b]], [rdst])
                            else:
                                tk.op("dve", lambda v: v.tensor_copy(out=dst_ap, in_=banks[b][:, :]),
                                      [rbank[b]], [rdst])
                        return f

                    def proj_v(slab, rsl, hh):
                        for g4 in range(NT5):
                            b = next_bank(0, 4)

                            def mm(t):
                                ins = None
                                for q in range(4):
                                    tb = g4 * 4 + q
                                    for kc in range(8):
                                        ins = t.matmul(banks[b][:, q * 128:(q + 1) * 128],
                                                       uT[:, kc, tb * 128:(tb + 1) * 128], slab[:, kc, :],
                                                       start=(kc == 0), stop=(kc == 7))
                                return ins
                            tk.op("pe", mm, [rsl] + r_uT[g4 * 4:g4 * 4 + 4], [rbank[b]])
                            src_ps = banks[b][:].rearrange("p (a b) -> p a b", a=4, b=128)
                            dst = Vt[:, g4 * 4:g4 * 4 + 4, hh, :]
                            k = ctr["ev"]
                            ctr["ev"] += 1
                            if k % 2 == 0:
                                tk.op("act", lambda a, s=src_ps, d=dst: a.activation(out=d, in_=s, func=AF.Copy),
                                      [rbank[b]], [r_v[g4]])
                            else:
                                tk.op("dve", lambda v, s=src_ps, d=dst: v.tensor_copy(out=d, in_=s),
                                      [rbank[b]], [r_v[g4]])

                    units = [("ret", 0), ("ret", 1)] + [("diff", h_) for h_ in range(4)]
                    for kind, ui in units:
                        is_ret = kind == "ret"
                        new_path = is_ret or ui > 0
                        RET_NEW = os.environ.get('KDBG_RET_NEW', '1') == '1'
                        DIFF_NEW = os.environ.get('KDBG_DIFF_NEW', '1') == '1'
                        for s_ in range(2 if is_ret else 1):
                            if is_ret:
                                lg = math.log(ret_gamma(2 * ui + s_))
                                bias = math.log(0.125)
                            else:
                                lg = -alibi_slope(ui)
                                bias = 0.0
                            if is_ret or ui == 0 or not DIFF_NEW:
                                tk.op("act", lambda a, s_=s_, lg=lg, bias=bias: a.activation(
                                    out=Mt[s_][:], in_=dmc[:], func=AF.Exp, scale=lg, bias=bias),
                                    [r_const], [r_dec[s_]])
                                tk.op("pool", lambda g, s_=s_: g.tensor_tensor(out=Mt[s_][:], in0=Mt[s_][:],
                                                                               in1=maskm[:], op=ALU.mult),
                                      [r_const, r_dec[s_]], [r_dec[s_]])
                            if (not is_ret) and (ui == 0 or not DIFF_NEW):
                                tk.op("act", lambda a, s_=s_, lg=lg, bias=bias: a.activation(
                                    out=Ft[s_][:], in_=dfull[:], func=AF.Exp, scale=lg, bias=bias),
                                    [r_const], [r_dec[s_]])
                            if is_ret:
                                tk.op("act", lambda a, s_=s_, lg=lg, bias=bias: a.activation(
                                    out=tabT[s_][:], in_=cdt[:], func=AF.Exp, scale=-lg, bias=bias),
                                    [r_const], [r_tab])
                                tk.op("dve", lambda v, s_=s_, lg=lg: v.memset(lgcol[64 * s_:64 * s_ + 64, :], lg),
                                      [], [r_tab])
                        if is_ret:
                            tk.op("act", lambda a: a.activation(out=Bt[:], in_=cqi[:], func=AF.Exp,
                                                                scale=lgcol[:, 0:1]),
                                  [r_const, r_tab], [r_tab])
                        elif ui > 0:
                            tk.op("dve", lambda v: v.tensor_scalar(out=tabT[0][:], in0=cdt[:],
                                                                   scalar1=float(alibi_slope(ui)), scalar2=None,
                                                                   op0=ALU.mult),
                                  [r_const], [r_tab])
                        if is_ret:
                            qc, kc_, vc, gc = OFF_RQ + 128 * ui, OFF_RK + 128 * ui, OFF_RV + 256 * ui, OFF_RG + 256 * ui
                        else:
                            qc, kc_, vc = OFF_DQ + 128 * ui, OFF_DK + 128 * ui, OFF_DV + 128 * ui
                        slab, rsl = load_slab(wcols(w_in[l], qc), key=nkey())
                        for t5 in range(NT5):
                            if is_ret:
                                def evq(b, t5=t5):
                                    tk.op("act", lambda a: a.activation(out=QT[:, t5 * 512:(t5 + 1) * 512],
                                                                        in_=banks[b][:, :], func=AF.Copy),
                                          [rbank[b]], [r_q[t5]])
                                    tk.op("dve", lambda v: v.tensor_tensor(out=QS[:, t5 * 512:(t5 + 1) * 512],
                                                                           in0=banks[b][:, :], in1=Bt[:], op=ALU.mult),
                                          [r_tab], [rbank[b], r_qs[t5]])
                                proj_fm(slab, rsl, t5, evq)
                            else:
                                proj_fm(slab, rsl, t5, evac_copy(QT[:, t5 * 512:(t5 + 1) * 512], r_q[t5]))
                        flush()
                        slab, rsl = load_slab(wcols(w_in[l], kc_), key=nkey())
                        for t5 in range(NT5):
                            proj_fm(slab, rsl, t5, evac_copy(KT[:, t5 * 512:(t5 + 1) * 512], r_k[t5]))
                        for hh in range(2 if is_ret else 1):
                            slab, rsl = load_slab(wcols(w_in[l], vc + 128 * hh), key=nkey())
                            proj_v(slab, rsl, hh)
                        if is_ret:
                            for hh in range(2):
                                slab, rsl = load_slab(wcols(w_in[l], gc + 128 * hh), key=nkey())
                                for t5 in range(NT5):
                                    def ev(b, hh=hh, t5=t5):
                                        tk.op("act", lambda a: a.activation(
                                            out=SG[:, hh, t5 * 512:(t5 + 1) * 512], in_=banks[b][:, :], func=AF.Silu),
                                            [rbank[b]], [r_sg[t5]])
                                    proj_fm(slab, rsl, t5, ev)

                        ZB = [6, 7]
                        for t5 in range(NT5):
                            OB = [6, 7] if (is_ret and t5 % 2 == 1) else [4, 5]
                            nkb = 4 * (t5 + 1)
                            kb0 = max(0, 4 * t5 - 4) if ((not is_ret) and ui == 0) else 0
                            ring = {"i": 0}

                            def issue_S(b_):
                                sb_ = [(ring["i"] % 2) * 2, (ring["i"] % 2) * 2 + 1]
                                ring["i"] += 1
                                offd = (512 * t5 - 128 * b_) >= 128
                                Qsrc, rq_ = (QS, r_qs[t5]) if (is_ret and offd and RET_NEW) else (QT, r_q[t5])
                                for s_ in range(2):
                                    bk = sb_[s_]
                                    tk.op("pe", lambda t, bk=bk, s_=s_: t.matmul(
                                        banks[bk][:, :], KT[64 * s_:64 * s_ + 64, b_ * 128:(b_ + 1) * 128],
                                        Qsrc[64 * s_:64 * s_ + 64, t5 * 512:(t5 + 1) * 512], start=True, stop=True),
                                        [r_k[b_ // 4], rq_], [rbank[bk]])
                                return sb_

                            def make_P(b_, sb_):
                                delta = 512 * t5 - 128 * b_
                                offd = delta >= 128
                                m = delta // 128 + 3
                                ps = []
                                for s_ in range(2):
                                    ds_ = s_ if is_ret else 0
                                    pi = ectr["P"] % 6
                                    ectr["P"] += 1
                                    bk = sb_[s_]
                                    if is_ret:
                                        if offd and not RET_NEW:
                                            c = ret_gamma(2 * ui + s_) ** delta
                                            tk.op("dve", lambda v, bk=bk, pi=pi, c=c: v.scalar_tensor_tensor(
                                                out=Pb[pi][:], in0=banks[bk][:, :], scalar=float(c), in1=Ft[0][:, :],
                                                op0=ALU.mult, op1=ALU.mult),
                                                [rbank[bk], r_dec[0]], [r_P[pi]])
                                        elif offd:
                                            tk.op("act", lambda a, bk=bk, pi=pi, s_=s_: a.activation(
                                                out=Pb[pi][:], in_=banks[bk][:, :], func=AF.Copy,
                                                scale=tabT[s_][:, m:m + 1]),
                                                [rbank[bk], r_tab], [r_P[pi]])
                                        else:
                                            tile_ap = Mt[ds_][:, 384 + delta:384 + delta + 512]
                                            tk.op("dve", lambda v, bk=bk, pi=pi, tile_ap=tile_ap: v.tensor_tensor(
                                                out=Pb[pi][:], in0=banks[bk][:, :], in1=tile_ap, op=ALU.mult),
                                                [rbank[bk], r_dec[ds_]], [r_P[pi]])
                                    elif ui == 0 or not DIFF_NEW:
                                        tile_ap = Ft[0][:, :] if offd else Mt[0][:, 384 + delta:384 + delta + 512]
                                        cb = -alibi_slope(ui) * delta if offd else 0.0
                                        ei = ectr["E"] % 4
                                        ectr["E"] += 1
                                        tk.op("act", lambda a, bk=bk, ei=ei, cb=cb: a.activation(
                                            out=Eb[ei][:], in_=banks[bk][:, :], func=AF.Exp, scale=0.125, bias=float(cb)),
                                            [rbank[bk]], [r_E[ei]])
                                        tk.op("dve", lambda v, ei=ei, pi=pi, tile_ap=tile_ap: v.tensor_tensor(
                                            out=Pb[pi][:], in0=Eb[ei][:], in1=tile_ap, op=ALU.mult),
                                            [r_E[ei], r_dec[0]], [r_P[pi]])
                                    else:
                                        if offd:
                                            tk.op("act", lambda a, bk=bk, pi=pi: a.activation(
                                                out=Pb[pi][:], in_=banks[bk][:, :], func=AF.Exp, scale=0.125,
                                                bias=tabT[0][:, m:m + 1]),
                                                [rbank[bk], r_tab], [r_P[pi]])
                                        else:
                                            ei = ectr["E"] % 4
                                            ectr["E"] += 1
                                            tk.op("act", lambda a, bk=bk, ei=ei: a.activation(
                                                out=Eb[ei][:], in_=banks[bk][:, :], func=AF.Exp, scale=0.125,
                                                bias=tabT[0][:, m:m + 1]),
                                                [rbank[bk], r_tab], [r_E[ei]])
                                            mk = maskm[:, 384 + delta:384 + delta + 512]
                                            tk.op("dve", lambda v, ei=ei, pi=pi, mk=mk: v.tensor_tensor(
                                                out=Pb[pi][:], in0=Eb[ei][:], in1=mk, op=ALU.mult),
                                                [r_E[ei], r_const], [r_P[pi]])
                                    ps.append(pi)
                                return ps

                            def issue_PV(b_, ps):
                                first, last = b_ == kb0, b_ == nkb - 1
                                for s_ in range(2):
                                    pi = ps[s_]
                                    vh = s_ if is_ret else 0
                                    if not is_ret:
                                        tk.op("pe", lambda t, s_=s_, pi=pi: t.matmul(
                                            banks[ZB[s_]][:, :], ones_b[:, :], Pb[pi][:, :], start=first, stop=last),
                                            [r_P[pi], r_const], [rbank[ZB[s_]]])
                                    tk.op("pe", lambda t, s_=s_, pi=pi, vh=vh: t.matmul(
                                        banks[OB[s_]][:, :], Vt[:, b_, vh, :], Pb[pi][:, :], start=first, stop=last),
                                        [r_P[pi], r_v[b_ // 4]], [rbank[OB[s_]]])

                            prev = None
                            for b_ in range(kb0, nkb):
                                sb_ = issue_S(b_)
                                if prev is not None:
                                    issue_PV(*prev)
                                ps = make_P(b_, sb_)
                                prev = (b_, ps)
                                if 1 <= b_ - kb0 <= 5:
                                    advance()
                            issue_PV(*prev)

                            qs = slice(t5 * 512, (t5 + 1) * 512)
                            tbs = r_mix[t5 * 4:t5 * 4 + 4]
                            if is_ret:
                                for s_ in range(2):
                                    ob = OB[s_]
                                    e0 = ep[3 * s_]
                                    tk.op("act", lambda a, ob=ob, e0=e0: a.activation(out=e0[:], in_=banks[ob][:, :],
                                                                                      func=AF.Square),
                                          [rbank[ob]], [r_ep[3 * s_]])
                                    pending.append(ret_part2(s_, 2 * ui + s_, ob, qs, tbs, r_sg[t5], l))
                            else:
                                e0, e1, e2, e3, e4, e5 = ep
                                tk.op("dve", lambda v: v.tensor_copy(out=e2[:], in_=banks[OB[0]][:, :]),
                                      [rbank[OB[0]]], [r_ep[2]])
                                tk.op("dve", lambda v: v.tensor_copy(out=e3[:], in_=banks[OB[1]][:, :]),
                                      [rbank[OB[1]]], [r_ep[3]])
                                tk.op("act", lambda a: a.activation(out=e0[:], in_=banks[ZB[0]][:, :], func=AF.Ln),
                                      [rbank[ZB[0]]], [r_ep[0]])
                                tk.op("act", lambda a: a.activation(out=e1[:], in_=banks[ZB[1]][:, :], func=AF.Ln),
                                      [rbank[ZB[1]]], [r_ep[1]])
                                pending.append(diff_part2(4 + ui, qs, tbs, l))
                    flush()

                    if True:
                        tk.dma(gbc[:], ffn_bc[l], [], [r_gbc], d_gbc)
                        for tb in range(NTB):
                            i = ctr["hb"] % NHB
                            ctr["hb"] += 1
                            tk.dma(hb[i][:], blk(hsrc, tb), [], [r_hb[i]], d_hb[i])
                            for nh in range(2):
                                b = next_bank()

                                def mm(t, b=b, nh=nh):
                                    ins = None
                                    for kc in range(8):
                                        ins = t.matmul(banks[b][:, :], mixT[:, kc, tb * 128:(tb + 1) * 128],
                                                       Wo[:, kc, nh * 512:(nh + 1) * 512], start=(kc == 0), stop=(kc == 7))
                                    return ins
                                tk.op("pe", mm, [r_wo, r_mix[tb]], [rbank[b]])
                                tk.op("dve", lambda v, b=b, nh=nh: v.tensor_tensor(
                                    out=hb[i][:, nh * 512:(nh + 1) * 512], in0=hb[i][:, nh * 512:(nh + 1) * 512],
                                    in1=banks[b][:, :], op=ALU.add),
                                    [rbank[b], r_hb[i]], [r_hb[i]])
                            tk.dma(blk(y[seq], tb), hb[i][:], [r_hb[i]], [], d_hst[i])
                            fused_a(i, tb)
                            if tb > 1:
                                stage_b(tb - 2)
                        stage_b(NTB - 2)
                        stage_b(NTB - 1)
                        tk.barrier()
                    tk.barrier()

                with ExitStack() as ff:
                    HT = S // 2
                    gT = ff.enter_context(nc.sbuf_tensor("gT_%d_%d" % (seq, l), [128, NJ, HT], BF16))
                    r_g = [R() for _ in range(NTB)]
                    Wd = ff.enter_context(nc.sbuf_tensor("Wd_%d_%d" % (seq, l), [128, NJ, D], BF16))
                    r_wd = R()
                    sgb = [ff.enter_context(nc.sbuf_tensor("sgb%d_%d_%d" % (i, seq, l), [128, 512], F32))
                           for i in range(2)]
                    r_sgb = [R(), R()]
                    k_sg = 0
                    last = (l == depth - 1)
                    if last:
                        tk.dma(gbc[:], final_bc[:, :], [], [r_gbc], d_gbc)
                    else:
                        tk.dma(gbc[:], attn_bc[l + 1], [], [r_gbc], d_gbc)
                    wd_flat = Wd[:].rearrange("p a b -> p (a b)")
                    for hf in range(2):
                        if hf == 1 and seq == 0:
                            tk.dma(wdscr[l], wd_flat, [r_wd], [], d_big)
                        for j in range(NJ):
                            slg, rg_ = load_slab(wcols(w_gate[l], j * 128), "act", key=l * NSLAB + 24 + j)
                            slu, ru_ = load_slab(wcols(w_up[l], j * 128), "dve", key=l * NSLAB + 46 + j)
                            if seq > 0 and hf == 0 and j == 1:
                                tk.dma(wd_flat, wdscr[l], [], [r_wd], d_big)
                            if hf == 0 and seq == 0:
                                st, rst = stage(w_down[l][j * 128:(j + 1) * 128, :])
                                cast("pool", Wd[:, j, :], st[:], [rst], [r_wd])
                            for t5 in (2 * hf, 2 * hf + 1):
                                bg = next_bank()
                                bu = next_bank()

                                def mmg(t, bb=bg, slab=slg, t5=t5):
                                    ins = None
                                    for kc in range(8):
                                        ins = t.matmul(banks[bb][:, :], slab[:, kc, :],
                                                       uT[:, kc, t5 * 512:(t5 + 1) * 512],
                                                       start=(kc == 0), stop=(kc == 7))
                                    return ins
                                tk.op("pe", mmg, [rg_] + r_uT[t5 * 4:t5 * 4 + 4], [rbank[bg]])

                                def mmu(t, bb=bu, slab=slu, t5=t5):
                                    ins = None
                                    for kc in range(8):
                                        ins = t.matmul(banks[bb][:, :], slab[:, kc, :],
                                                       uT[:, kc, t5 * 512:(t5 + 1) * 512],
                                                       start=(kc == 0), stop=(kc == 7))
                                    return ins
                                tk.op("pe", mmu, [ru_] + r_uT[t5 * 4:t5 * 4 + 4], [rbank[bu]])
                                si = k_sg % 2
                                k_sg += 1
                                tk.op("act", lambda a, si=si, bg=bg: a.activation(out=sgb[si][:], in_=banks[bg][:, :],
                                                                                  func=AF.Silu),
                                      [rbank[bg]], [r_sgb[si]])
                                lo = (t5 - 2 * hf) * 512
                                tk.op("dve", lambda v, si=si, bu=bu, j=j, lo=lo: v.tensor_tensor(
                                    out=gT[:, j, lo:lo + 512], in0=sgb[si][:], in1=banks[bu][:, :], op=ALU.mult),
                                    [r_sgb[si], rbank[bu]], r_g[t5 * 4:t5 * 4 + 4])
                        for tb in range(8 * hf, 8 * hf + 8):
                            i = ctr["hb"] % NHB
                            ctr["hb"] += 1
                            tk.dma(hb[i][:], blk(y[seq], tb), [], [r_hb[i]], d_hb[i])
                            lo = (tb - 8 * hf) * 128
                            for nh in range(2):
                                b = next_bank()

                                def mm(t, b=b, nh=nh, lo=lo):
                                    ins = None
                                    for j in range(NJ):
                                        ins = t.matmul(banks[b][:, :], gT[:, j, lo:lo + 128],
                                                       Wd[:, j, nh * 512:(nh + 1) * 512],
                                                       start=(j == 0), stop=(j == NJ - 1))
                                    return ins
                                tk.op("pe", mm, [r_wd, r_g[tb]], [rbank[b]])
                                tk.op("dve", lambda v, b=b, nh=nh: v.tensor_tensor(
                                    out=hb[i][:, nh * 512:(nh + 1) * 512], in0=hb[i][:, nh * 512:(nh + 1) * 512],
                                    in1=banks[b][:, :], op=ALU.add),
                                    [rbank[b], r_hb[i]], [r_hb[i]])
                            if last:
                                norm_stats(i, tb)
                                tk.op("dve", lambda v: v.scalar_tensor_tensor(
                                    out=hb[i][:], in0=hb[i][:], scalar=stat[:, 2 * NTB + tb:2 * NTB + tb + 1],
                                    in1=gbc[:], op0=ALU.mult, op1=ALU.mult),
                                    [r_hb[i], r_stat[tb], r_gbc], [r_hb[i]])
                            tok = tk.dma(blk(y[seq], tb), hb[i][:], [r_hb[i]], [], d_hst[i])
                            if last:
                                out_tokens.append(tok)
                            else:
                                fused_a(i, tb)
                                if tb > 8 * hf + 1:
                                    stage_b(tb - 2)
                        if not last:
                            stage_b(8 * hf + 6)
                            stage_b(8 * hf + 7)
                    tk.barrier()
                tk.barrier()
    tk.finish(out_tokens)
    tk.barrier()
    es.close()
    return nc


def host_consts():
    p = np.arange(128, dtype=np.float32)[:, None]
    c = np.arange(896, dtype=np.float32)[None, :]
    dm = (c - 384.0 - p).astype(np.float32)
    df = (np.arange(512, dtype=np.float32)[None, :] - p).astype(np.float32)
    ident = np.eye(128, dtype=np.float32)
    dt_ = (p - 128.0 * (np.arange(16, dtype=np.float32)[None, :] - 3.0)).astype(np.float32)
    qi = np.ascontiguousarray(np.broadcast_to(np.arange(512, dtype=np.float32)[None, :], (128, 512)))
    return ident, dm, df, dt_, qi


_CACHE = {}


def run(inputs, nseq, depth, ncores, trace=False):
    key = (nseq, depth)
    if key not in _CACHE:
        _CACHE[key] = build_program(nseq, depth)
    nc = _CACHE[key]
    f = lambda a: np.ascontiguousarray(np.asarray(a, dtype=np.float32))
    ident, dm, df, dt_, qi_ = host_consts()
    bc = lambda a: np.ascontiguousarray(np.broadcast_to(f(a)[:, None, :], (a.shape[0], 128, a.shape[1])))
    lam = np.concatenate([f(inputs["lambda_q1"])[:, None, :], f(inputs["lambda_k1"])[:, None, :],
                          f(inputs["lambda_q2"])[:, None, :], f(inputs["lambda_k2"])[:, None, :]], axis=1)
    lam = lam[:depth].reshape(1, depth * 4 * 64)
    shared = {
        "w_in": f(inputs["w_in"])[:depth], "w_out": f(inputs["w_out"])[:depth],
        "w_gate": f(inputs["w_gate"])[:depth], "w_up": f(inputs["w_up"])[:depth],
        "w_down": f(inputs["w_down"])[:depth],
        "attn_bc": bc(f(inputs["attn_norm"])[:depth]), "ffn_bc": bc(f(inputs["ffn_norm"])[:depth]),
        "final_bc": np.ascontiguousarray(np.broadcast_to(f(inputs["final_norm"])[None, :], (128, D))),
        "retn_col": np.ascontiguousarray(f(inputs["ret_norm"])[:depth].T),
        "difn_col": np.ascontiguousarray(f(inputs["diff_norm"])[:depth].T),
        "lam_in": np.ascontiguousarray(np.broadcast_to(lam, (128, depth * 4 * 64))),
        "c_ident": ident, "c_dm": dm, "c_df": df, "c_dt": dt_, "c_qi": qi_,
    }
    xs = f(inputs["x"])
    in_maps = []
    for c in range(ncores):
        m = dict(shared)
        m["x"] = np.ascontiguousarray(xs[c * nseq:(c + 1) * nseq])
        in_maps.append(m)
    res = run_bass_kernel_spmd(nc, in_maps, core_ids=list(range(ncores)), **({"trace": True} if trace else {}))
    out = np.concatenate([r["y"] for r in res.results], axis=0)
    return out, res


def kernel(x, attn_norm, w_in, ret_norm, lambda_q1, lambda_k1, lambda_q2, lambda_k2,
           diff_norm, w_out, ffn_norm, w_gate, w_up, w_down, final_norm):
    inputs = dict(x=x, attn_norm=attn_norm, w_in=w_in, ret_norm=ret_norm, lambda_q1=lambda_q1,
                  lambda_k1=lambda_k1, lambda_q2=lambda_q2, lambda_k2=lambda_k2, diff_norm=diff_norm,
                  w_out=w_out, ffn_norm=ffn_norm, w_gate=w_gate, w_up=w_up, w_down=w_down,
                  final_norm=final_norm)
    out, _ = run(inputs, BATCH // NCORES, DEPTH, NCORES)
    return out.astype(np.float32)
```
